# Optimizing a Trainium2 kernel written in Bass

```python
import math
import jax, jax.numpy as jnp
from jax import lax
import numpy as np

D_MODEL = 1024
BATCH = 4
SEQ = 8192
DEPTH = 1

GDN_HEADS = 4
GDN_DK = 128
GDN_DV = 128
GDN_CONV = 4
GDN_CHUNK = 64
DIFF_HEADS = 4
DIFF_DH = 64
Q_BLOCK = 128
REL_BUCKETS = 32
REL_MAX_DIST = 128
N_EXPERTS = 32
TOP_K = 4
D_EXPERT = 1024
SWIGLU_LIMIT = 7.0
SWIGLU_ALPHA = 1.702
EXPERT_BLOCK = 256
PLE_DIM = 256
DEEPNORM_ALPHA = (2 * DEPTH) ** 0.25
DEEPNORM_BETA = (8 * DEPTH) ** -0.25
LN_EPS = 1e-5
RMS_EPS = 1e-6

GDN_QK_W = GDN_HEADS * GDN_DK
GDN_V_W = GDN_HEADS * GDN_DV
DIFF_QK_W = DIFF_HEADS * 2 * DIFF_DH
DIFF_V_W = DIFF_HEADS * 2 * DIFF_DH
MIX_W = GDN_V_W + DIFF_V_W
IN_SPLITS = (GDN_QK_W, GDN_QK_W, GDN_V_W, GDN_HEADS, GDN_HEADS, GDN_V_W, DIFF_QK_W, DIFF_QK_W, DIFF_V_W)
D_IN_PROJ = sum(IN_SPLITS)

kernel_name = 'hymba_gdn_diffattn_moe_deepnorm'


def split_cols(t, sizes):
    offs = []
    acc = 0
    for s in sizes[:-1]:
        acc += s
        offs.append(acc)
    return jnp.split(t, offs, axis=-1)


def layer_norm(x, g, b):
    xf = x.astype(jnp.float32)
    mu = jnp.mean(xf, axis=-1, keepdims=True)
    var = jnp.mean(jnp.square(xf - mu), axis=-1, keepdims=True)
    y = (xf - mu) * lax.rsqrt(var + LN_EPS) * g.astype(jnp.float32) + b.astype(jnp.float32)
    return y.astype(x.dtype)


def rms_norm(x, w):
    xf = x.astype(jnp.float32)
    return xf * lax.rsqrt(jnp.mean(xf * xf, axis=-1, keepdims=True) + RMS_EPS) * w.astype(jnp.float32)


def l2_normalize(t):
    return t * lax.rsqrt(jnp.sum(t * t, axis=-1, keepdims=True) + 1e-6)


def causal_depthwise_conv(x, w):
    K, C = w.shape
    return lax.conv_general_dilated(x, w[:, None, :].astype(x.dtype), window_strides=(1,),
                                    padding=[(K - 1, 0)], dimension_numbers=('NWC', 'WIO', 'NWC'),
                                    feature_group_count=C)


def t5_bucket(dist):
    max_exact = REL_BUCKETS // 2
    d = jnp.maximum(dist, 0)
    large = max_exact + (jnp.log(jnp.maximum(d, 1).astype(jnp.float32) / max_exact)
                         / math.log(REL_MAX_DIST / max_exact) * (REL_BUCKETS - max_exact)).astype(jnp.int32)
    large = jnp.minimum(large, REL_BUCKETS - 1)
    return jnp.where(d < max_exact, d, large)


def gated_delta_rule_chunked(q, k, v, g, beta):
    B, S, H, dk = q.shape
    dv = v.shape[-1]
    C = GDN_CHUNK
    n = S // C
    f32 = jnp.float32

    def chunk(t):
        t = t.astype(f32).reshape((B, n, C, H) + t.shape[3:])
        return jnp.moveaxis(t, 3, 1)

    q = chunk(l2_normalize(q.astype(f32)) * dk ** -0.5)
    k = chunk(l2_normalize(k.astype(f32)))
    v = chunk(v)
    g = jnp.cumsum(chunk(g), axis=-1)
    beta = chunk(beta)
    lower = jnp.tril(jnp.ones((C, C), dtype=bool))
    strict = jnp.tril(jnp.ones((C, C), dtype=bool), k=-1)
    gdiff = g[..., :, None] - g[..., None, :]
    decay = jnp.where(lower, jnp.exp(jnp.where(lower, gdiff, 0.0)), 0.0)
    kb = k * beta[..., None]
    L = jnp.where(strict, jnp.einsum('bhnid,bhnjd->bhnij', kb, k) * decay, 0.0)
    eye = jnp.eye(C, dtype=f32)
    rhs = jnp.concatenate([v * beta[..., None], kb * jnp.exp(g)[..., None]], axis=-1)
    sol = lax.linalg.triangular_solve(eye + L, rhs, left_side=True, lower=True, unit_diagonal=True)
    u, w = sol[..., :dv], sol[..., dv:]
    intra = jnp.einsum('bhnid,bhnjd->bhnij', q, k) * decay
    q_dec = q * jnp.exp(g)[..., None]
    k_dec = k * jnp.exp(g[..., -1:] - g)[..., None]
    g_last = jnp.exp(g[..., -1])

    def step(state, inp):
        q_i, k_i, u_i, w_i, a_i, gl = inp
        v_new = u_i - jnp.einsum('bhck,bhkv->bhcv', w_i, state)
        o = jnp.einsum('bhck,bhkv->bhcv', q_i, state) + jnp.einsum('bhcj,bhjv->bhcv', a_i, v_new)
        state = state * gl[..., None, None] + jnp.einsum('bhck,bhcv->bhkv', k_i, v_new)
        return state, o

    xs = tuple(jnp.moveaxis(t, 2, 0) for t in (q_dec, k_dec, u, w, intra, g_last))
    _, o = lax.scan(step, jnp.zeros((B, H, dk, dv), f32), xs)
    return jnp.transpose(o, (1, 0, 3, 2, 4)).reshape(B, S, H, dv)


def diff_attention_blocked(q1, q2, k1, k2, v, lam, rel_bias):
    B, S, H, dh = q1.shape
    nblk = S // Q_BLOCK
    scale = dh ** -0.5
    dist_bias = rel_bias[t5_bucket(jnp.arange(S))].astype(jnp.float32)
    k_pos = jnp.arange(S)

    def to_blocks(t):
        return jnp.moveaxis(t.reshape(B, nblk, Q_BLOCK, H, t.shape[-1]), 1, 0)

    def block(args):
        bi, qa, qb = args
        q_pos = bi * Q_BLOCK + jnp.arange(Q_BLOCK)
        rel = q_pos[:, None] - k_pos[None, :]
        causal = rel >= 0
        bias = jnp.transpose(dist_bias[jnp.clip(rel, 0, S - 1)], (2, 0, 1))

        def probs(qx, kx):
            s = jnp.einsum('bqhd,bkhd->bhqk', qx, kx).astype(jnp.float32) * scale + bias
            return jax.nn.softmax(jnp.where(causal, s, -jnp.inf), axis=-1)

        a = probs(qa, k1) - lam * probs(qb, k2)
        return jnp.einsum('bhqk,bkhe->bqhe', a.astype(v.dtype), v)

    out = lax.map(block, (jnp.arange(nblk), to_blocks(q1), to_blocks(q2)))
    return jnp.moveaxis(out, 0, 1).reshape(B, S, H, v.shape[-1])


def moe_ffn(h, router_w, router_b, w_gate_up, b_gate_up, w_down, b_down):
    B, S, D = h.shape
    N = B * S
    A = N * TOP_K
    xf = h.reshape(N, D)
    logits = (xf @ router_w).astype(jnp.float32) + router_b.astype(jnp.float32)
    top_val, top_idx = lax.top_k(logits, TOP_K)
    gates = jax.nn.softmax(top_val, axis=-1)
    e_flat = top_idx.reshape(A)
    tok_flat = jnp.arange(A, dtype=jnp.int32) // TOP_K
    order = jnp.argsort(e_flat)
    e_sorted = e_flat[order]
    counts = jnp.bincount(e_flat, length=N_EXPERTS)
    padded = (counts + EXPERT_BLOCK - 1) // EXPERT_BLOCK * EXPERT_BLOCK
    start = jnp.cumsum(counts) - counts
    pend = jnp.cumsum(padded)
    pstart = pend - padded
    dest = pstart[e_sorted] + jnp.arange(A, dtype=jnp.int32) - start[e_sorted]
    n_blocks = -(-A // EXPERT_BLOCK) + N_EXPERTS
    P = n_blocks * EXPERT_BLOCK
    tok_buf = jnp.full((P,), N, dtype=jnp.int32).at[dest].set(tok_flat[order])
    gate_buf = jnp.zeros((P,), jnp.float32).at[dest].set(gates.reshape(A)[order])
    blk_expert = jnp.minimum(jnp.searchsorted(pend, jnp.arange(n_blocks) * EXPERT_BLOCK, side='right'),
                             N_EXPERTS - 1)
    x_pad = jnp.concatenate([xf, jnp.zeros((1, D), xf.dtype)], axis=0)

    def expert_block(args):
        tok_b, e = args
        gu = x_pad[tok_b] @ w_gate_up[e] + b_gate_up[e]
        gate = jnp.minimum(gu[:, :D_EXPERT], SWIGLU_LIMIT)
        up = jnp.clip(gu[:, D_EXPERT:], -SWIGLU_LIMIT, SWIGLU_LIMIT)
        act = (up + 1.0) * gate * jax.nn.sigmoid(SWIGLU_ALPHA * gate)
        return act @ w_down[e] + b_down[e]

    y_blocks = lax.map(expert_block, (tok_buf.reshape(n_blocks, EXPERT_BLOCK), blk_expert))
    y = jax.ops.segment_sum(y_blocks.reshape(P, D) * gate_buf[:, None].astype(y_blocks.dtype), tok_buf,
                            num_segments=N + 1)
    return y[:N].reshape(B, S, D)


def setup_inputs(seed: int = 0) -> dict:
    key = jax.random.key(seed)
    ks = jax.random.split(key, 29)
    f32 = jnp.float32
    nrm = lambda k, shape, s: jax.random.normal(k, shape, f32) * s
    dt = jnp.exp(jax.random.uniform(ks[5], (DEPTH, GDN_HEADS), f32, math.log(1e-3), math.log(1e-1)))
    return {
        'x': nrm(ks[0], (BATCH, SEQ, D_MODEL), 1.0),
        'p': nrm(ks[1], (DEPTH, BATCH, SEQ, PLE_DIM), 1.0),
        'w_in': nrm(ks[2], (DEPTH, D_MODEL, D_IN_PROJ), D_MODEL ** -0.5),
        'conv_w': nrm(ks[3], (DEPTH, GDN_CONV, 2 * GDN_QK_W + GDN_V_W), GDN_CONV ** -0.5),
        'gdn_a_log': jnp.log(jax.random.uniform(ks[4], (DEPTH, GDN_HEADS), f32, 1.0, 16.0)),
        'gdn_dt_bias': dt + jnp.log(-jnp.expm1(-dt)),
        'gdn_norm_w': 1.0 + nrm(ks[6], (DEPTH, GDN_DV), 0.02),
        'diff_lq1': nrm(ks[7], (DEPTH, DIFF_DH), 0.1),
        'diff_lk1': nrm(ks[8], (DEPTH, DIFF_DH), 0.1),
        'diff_lq2': nrm(ks[9], (DEPTH, DIFF_DH), 0.1),
        'diff_lk2': nrm(ks[10], (DEPTH, DIFF_DH), 0.1),
        'diff_norm_w': 1.0 + nrm(ks[11], (DEPTH, 2 * DIFF_DH), 0.02),
        'w_out': nrm(ks[12], (DEPTH, MIX_W, D_MODEL), MIX_W ** -0.5 * DEEPNORM_BETA),
        'rel_bias': nrm(ks[13], (REL_BUCKETS, DIFF_HEADS), 0.3),
        'ln1_g': 1.0 + nrm(ks[14], (DEPTH, D_MODEL), 0.02),
        'ln1_b': nrm(ks[15], (DEPTH, D_MODEL), 0.02),
        'router_w': nrm(ks[16], (DEPTH, D_MODEL, N_EXPERTS), D_MODEL ** -0.5),
        'router_b': nrm(ks[17], (DEPTH, N_EXPERTS), 0.01),
        'w_gate_up': nrm(ks[18], (DEPTH, N_EXPERTS, D_MODEL, 2 * D_EXPERT), D_MODEL ** -0.5),
        'b_gate_up': nrm(ks[19], (DEPTH, N_EXPERTS, 2 * D_EXPERT), 0.01),
        'w_down': nrm(ks[20], (DEPTH, N_EXPERTS, D_EXPERT, D_MODEL), D_EXPERT ** -0.5 * DEEPNORM_BETA),
        'b_down': nrm(ks[21], (DEPTH, N_EXPERTS, D_MODEL), 0.01),
        'ln2_g': 1.0 + nrm(ks[22], (DEPTH, D_MODEL), 0.02),
        'ln2_b': nrm(ks[23], (DEPTH, D_MODEL), 0.02),
        'ple_gate_w': nrm(ks[24], (DEPTH, D_MODEL, D_MODEL), D_MODEL ** -0.5),
        'ple_gate_b': nrm(ks[25], (DEPTH, D_MODEL), 0.01),
        'ple_proj_w': nrm(ks[26], (DEPTH, PLE_DIM, D_MODEL), PLE_DIM ** -0.5 * DEEPNORM_BETA),
        'ln3_g': 1.0 + nrm(ks[27], (DEPTH, D_MODEL), 0.02),
        'ln3_b': nrm(ks[28], (DEPTH, D_MODEL), 0.02),
    }


def reference(x, p, w_in, conv_w, gdn_a_log, gdn_dt_bias, gdn_norm_w, diff_lq1, diff_lk1, diff_lq2,
              diff_lk2, diff_norm_w, w_out, rel_bias, ln1_g, ln1_b, router_w, router_b, w_gate_up,
              b_gate_up, w_down, b_down, ln2_g, ln2_b, ple_gate_w, ple_gate_b, ple_proj_w, ln3_g, ln3_b):
    B, S, _ = x.shape
    f32 = jnp.float32
    for i in range(DEPTH):
        lam_init = 0.8 - 0.6 * math.exp(-0.3 * i)
        proj = x @ w_in[i]
        gq, gk, gv, ga, gb, gz, dq, dk, dv = split_cols(proj, IN_SPLITS)
        qkv = jax.nn.silu(causal_depthwise_conv(jnp.concatenate([gq, gk, gv], axis=-1), conv_w[i]))
        gq, gk, gv = split_cols(qkv, (GDN_QK_W, GDN_QK_W, GDN_V_W))
        g_log = -jnp.exp(gdn_a_log[i].astype(f32)) * jax.nn.softplus(ga.astype(f32) + gdn_dt_bias[i].astype(f32))
        beta = jax.nn.sigmoid(gb.astype(f32))
        o_gdn = gated_delta_rule_chunked(gq.reshape(B, S, GDN_HEADS, GDN_DK), gk.reshape(B, S, GDN_HEADS, GDN_DK),
                                         gv.reshape(B, S, GDN_HEADS, GDN_DV), g_log, beta)
        o_gdn = rms_norm(o_gdn, gdn_norm_w[i]) * jax.nn.silu(gz.reshape(B, S, GDN_HEADS, GDN_DV).astype(f32))
        dq = dq.reshape(B, S, DIFF_HEADS, 2, DIFF_DH)
        dk = dk.reshape(B, S, DIFF_HEADS, 2, DIFF_DH)
        lam = (jnp.exp(jnp.sum(diff_lq1[i].astype(f32) * diff_lk1[i].astype(f32)))
               - jnp.exp(jnp.sum(diff_lq2[i].astype(f32) * diff_lk2[i].astype(f32))) + lam_init)
        o_diff = diff_attention_blocked(dq[:, :, :, 0], dq[:, :, :, 1], dk[:, :, :, 0], dk[:, :, :, 1],
                                        dv.reshape(B, S, DIFF_HEADS, 2 * DIFF_DH), lam, rel_bias)
        o_diff = rms_norm(o_diff, diff_norm_w[i]) * (1.0 - lam_init)
        heads = jnp.concatenate([o_gdn.reshape(B, S, GDN_V_W), o_diff.reshape(B, S, DIFF_V_W)], axis=-1)
        mix = heads.astype(x.dtype) @ w_out[i]
        x = layer_norm(DEEPNORM_ALPHA * x + mix, ln1_g[i], ln1_b[i])
        ffn = moe_ffn(x, router_w[i], router_b[i], w_gate_up[i], b_gate_up[i], w_down[i], b_down[i])
        x = layer_norm(DEEPNORM_ALPHA * x + ffn, ln2_g[i], ln2_b[i])
        gate = jax.nn.sigmoid(x @ ple_gate_w[i] + ple_gate_b[i])
        x = layer_norm(DEEPNORM_ALPHA * x + gate * (p[i] @ ple_proj_w[i]), ln3_g[i], ln3_b[i])
    return x
```

```python
import math
from contextlib import ExitStack
import numpy as np
import concourse.bass as bass
import concourse.mybir as mybir
from concourse.alu_op_type import AluOpType as ALU
from concourse.bass_utils import run_bass_kernel_spmd

AF = mybir.ActivationFunctionType
F32 = mybir.dt.float32
BF16 = mybir.dt.bfloat16
I32 = mybir.dt.int32

ENGS = ('pe', 'act', 'pool', 'dve', 'sp')
SEM_LIMIT = 30000
RING = 8
RINGS = {'sp': 16, 'pool': 24, 'act': 8}


class Reg:
    __slots__ = ('w', 'pc', 'pd', 'rc', 'rd', 'excl')

    def __init__(self, excl=False):
        self.excl = excl
        self.w = None
        self.pc = {}
        self.pd = set()
        self.rc = {}
        self.rd = set()


class Sched:
    def __init__(self, nc, same_engine_sync=True):
        self.nc = nc
        self.e = {'pe': nc.tensor, 'act': nc.scalar, 'pool': nc.gpsimd, 'dve': nc.vector, 'sp': nc.sync}
        self.cnt = {k: 0 for k in ENGS}
        self.sems = {k: [] for k in ENGS}
        self.clock = {k: {f: 0 for f in ENGS} for k in ENGS}
        self.snap = {k: [] for k in ENGS}
        self.dknown = {k: set() for k in ENGS}
        self.dq = {q: {'n': 0, 'sems': []} for q in ('sp', 'pool', 'act')}
        self.same = same_engine_sync
        self.nwait = 0
        self.ninst = 0

    def _csem(self, eng, n):
        ep = (n - 1) // SEM_LIMIT
        while len(self.sems[eng]) <= ep:
            self.sems[eng].append(self.nc.alloc_semaphore(name=f"c_{eng}_{len(self.sems[eng])}"))
        return self.sems[eng][ep], (n - 1) % SEM_LIMIT + 1

    def _dsem(self, q, i):
        d = self.dq[q]
        R = RINGS[q]
        while len(d['sems']) < R:
            d['sems'].append(self.nc.alloc_semaphore(name=f"d_{q}_{len(d['sems'])}"))
        return d['sems'][i % R], 16 * (i // R + 1)

    def _wait(self, E, tok):
        if tok[0] == 'c':
            _, F, n = tok
            if F == E and (E == 'pe' or not self.same):
                return
            if self.clock[E][F] >= n:
                return
            sem, val = self._csem(F, n)
            self.e[E].wait_ge(sem, val)
            self.nwait += 1
            self.clock[E][F] = n
            sn = self.snap[F][n - 1]
            ce = self.clock[E]
            for g, v in sn.items():
                if v > ce[g]:
                    ce[g] = v
        else:
            _, q, i = tok
            if (q, i) in self.dknown[E]:
                return
            sem, val = self._dsem(q, i)
            self.e[E].wait_ge(sem, val)
            self.nwait += 1
            self.dknown[E].add((q, i))

    def _deps(self, E, reads, writes, pw):
        toks = []
        for r in reads:
            if r.w is not None:
                toks.append(r.w)
            for f, n in r.pc.items():
                toks.append(('c', f, n))
            toks.extend(r.pd)
        for w in writes:
            if w.w is not None:
                toks.append(w.w)
            for f, n in w.pc.items():
                toks.append(('c', f, n))
            toks.extend(w.pd)
            for f, n in w.rc.items():
                toks.append(('c', f, n))
            toks.extend(w.rd)
        for w in pw:
            if w.w is not None:
                toks.append(w.w)
            for f, n in w.rc.items():
                toks.append(('c', f, n))
            toks.extend(w.rd)
        for t in toks:
            self._wait(E, t)

    def _post(self, tok, reads, writes, pw):
        for r in reads:
            if tok[0] == 'c':
                if r.rc.get(tok[1], 0) < tok[2]:
                    r.rc[tok[1]] = tok[2]
            else:
                r.rd.add(tok)
        for w in writes:
            w.w = tok
            w.pc = {}
            w.pd = set()
            w.rc = {}
            w.rd = set()
        for w in pw:
            if tok[0] == 'c':
                if w.pc.get(tok[1], 0) < tok[2]:
                    w.pc[tok[1]] = tok[2]
            else:
                w.pd.add(tok)

    def op(self, E, fn, reads=(), writes=(), pw=()):
        if any(r.excl for r in reads) or any(r.excl for r in pw):
            writes = list(writes) + [r for r in reads if r.excl] + [r for r in pw if r.excl]
            reads = [r for r in reads if not r.excl]
            pw = [r for r in pw if not r.excl]
        self._deps(E, reads, writes, pw)
        inst = fn(self.e[E])
        n = self.cnt[E] + 1
        sem, _ = self._csem(E, n)
        inst.then_inc(sem, 1)
        self.cnt[E] = n
        self.snap[E].append(dict(self.clock[E]))
        self.ninst += 1
        tok = ('c', E, n)
        self._post(tok, reads, writes, pw)
        return tok

    def dma(self, q, fn, reads=(), writes=(), pw=()):
        E = q
        self._deps(E, reads, writes, pw)
        d = self.dq[q]
        i = d['n']
        if i >= RINGS[q]:
            self._wait(E, ('d', q, i - RINGS[q]))
        sem, _ = self._dsem(q, i)
        inst = fn(self.e[E])
        inst.then_inc(sem, 16)
        d['n'] = i + 1
        self.ninst += 1
        tok = ('d', q, i)
        self._post(tok, reads, writes, pw)
        return tok

    def barrier(self):
        for E in ENGS:
            for F in ENGS:
                if F != E and self.cnt[F] > 0:
                    self._wait(E, ('c', F, self.cnt[F]))
                elif F == E and E != 'pe' and self.cnt[F] > 0:
                    self._wait(E, ('c', F, self.cnt[F]))
            self.drain(E)

    def drain(self, E):
        for q in self.dq:
            d = self.dq[q]
            for i in range(max(0, d['n'] - RINGS[q]), d['n']):
                self._wait(E, ('d', q, i))


D = 1024
GH = 4
DH = 4
PLE = 256
NEXP = 32
DEXP = 1024
DINP = 3592
C_GQ, C_GK, C_GV, C_A, C_B, C_Z, C_DQ, C_DK, C_DV = 0, 512, 1024, 1536, 1540, 1544, 2056, 2568, 3080
ALPHA = 2 ** 0.25
LAM_INIT = 0.8 - 0.6 * math.exp(0.0)
NEG = -30000.0


def t5_bucket_np(d):
    d = np.maximum(d, 0)
    large = 16 + (np.log(np.maximum(d, 1).astype(np.float32) / np.float32(16)) / np.float32(math.log(8.0))
                  * np.float32(16)).astype(np.int32)
    large = np.minimum(large, 31)
    return np.where(d < 16, d, large)


def host_consts():
    i = np.arange(128)
    ch = i // 64
    c = {}
    c['ident'] = np.eye(128, dtype=np.float32)
    same = ch[:, None] == ch[None, :]
    tri = (same & (i[:, None] <= i[None, :])).astype(np.float32)
    chi = (ch[:, None] == np.arange(2)[None, :]).astype(np.float32)
    c['trichi'] = np.concatenate([tri, chi], axis=1)
    c['blk'] = same.astype(np.float32)
    c['negt'] = np.where(same & (i[None, :] >= i[:, None]), 0.0, NEG).astype(np.float32)
    c['smask'] = (1.0 - np.eye(128)).astype(np.float32)
    c['su'] = (i[:, None] < i[None, :]).astype(np.float32)
    oh = np.zeros((128, 64, 128), np.float32)
    for ty in range(2):
        dmat = i[None, :] - i[:, None] + 128 * ty
        bk = t5_bucket_np(dmat)
        for b in range(32):
            oh[:, ty * 32 + b, :] = ((bk == b) & (dmat >= 0)).astype(np.float32)
    c['t5oh'] = oh
    c['negd'] = np.where(i[None, :] >= i[:, None], 0.0, NEG).astype(np.float32)
    return c


def build(SH, CAP, stop_after=99, dbg=False):
    F32R = mybir.dt.float32r
    NT = SH // 128
    NTT = 2 * NT
    NSL = CAP // 128
    VW = 136
    assert NT % 4 == 0
    nc = bass.Bass("TRN2", target_bir_lowering=False)
    S = Sched(nc)

    def din(name, shape, dt=F32):
        return nc.dram_tensor(name, list(shape), dt, kind="ExternalInput").ap()

    def dscr(name, shape, dt):
        return nc.dram_tensor(name, list(shape), dt, kind=("ExternalOutput" if dbg else "Internal")).ap()

    x_all = din("x_all", [2 * SH, D])
    p_own = din("p_own", [SH, PLE])
    pmask_d = din("pmask", [128, 1])
    w_in = din("w_in", [D, DINP])
    conv_w = din("conv_w", [128, 12, 4])
    a_log = din("gdn_a_log", [1, 4]); dt_bias = din("gdn_dt_bias", [1, 4])
    gnw = din("gdn_norm_w", [1, 128]); dnw = din("diff_norm_w", [1, 128])
    lqk = [din(n, [1, 64]) for n in ("diff_lq1", "diff_lk1", "diff_lq2", "diff_lk2")]
    w_out = din("w_out", [D, D])
    rel_bias = din("rel_bias", [1, 128])
    ln_g = [din(f"ln{k}_g", [1, D]) for k in (1, 2, 3)]
    ln_b = [din(f"ln{k}_b", [1, D]) for k in (1, 2, 3)]
    router_w = din("router_w", [D, NEXP]); router_b = din("router_b", [1, NEXP])
    w_gu = din("w_gate_up", [NEXP, D, 2 * DEXP]); b_gu = din("b_gate_up", [128, NEXP * 16])
    w_dn = din("w_down", [NEXP, DEXP, D]); b_dn = din("b_down", [NEXP, D])
    pg_w = din("ple_gate_w", [D, D]); pg_b = din("ple_gate_b", [1, D]); pp_w = din("ple_proj_w", [PLE, D])
    c_ident = din("c_ident", [128, 128]); c_trichi = din("c_trichi", [128, 130]); c_blk = din("c_blk", [128, 128])
    c_negt = din("c_negt", [128, 128]); c_smask = din("c_smask", [128, 128]); c_su = din("c_su", [128, 128])
    c_t5oh = din("c_t5oh", [128, 64, 128]); c_negd = din("c_negd", [128, 128]); c_ecap = din("c_ecap", [1, NEXP])
    y_out = nc.dram_tensor("y_out", [SH, D], F32, kind="ExternalOutput").ap()

    DQs = dscr("s_dq", [4, 128, SH], BF16)
    DKs = dscr("s_dk", [4, 128, 2 * SH], BF16)
    DVs = dscr("s_dv", [2 * SH, 4 * VW], BF16)
    HDs = dscr("s_heads", [SH, D], BF16)
    X1s = dscr("s_x1", [SH, D], F32)
    XSs = dscr("s_xslots", [NEXP * CAP, D], BF16)
    YSs = dscr("s_yslots", [NEXP * CAP, D], BF16)
    r_dq = Reg(); r_dk = Reg(); r_dv = Reg()
    r_hd = [Reg() for _ in range(NT)]
    r_x1 = [Reg() for _ in range(NT)]
    r_xs = Reg()
    r_ys = Reg()
    r_yout = Reg()

    def R3(ap, b=128):
        return ap.rearrange("p (a b) -> p a b", b=b)

    with ExitStack() as G:
        def sbt(st, name, shape, dt):
            return st.enter_context(nc.sbuf_tensor(name, list(shape), dt))

        def pst(st, name, shape, dt):
            return st.enter_context(nc.psum_tensor(name, list(shape), dt))

        r_c = Reg()
        RC = [r_c]

        def cload(q, out_ap, in_ap, **kw):
            S.dma(q, lambda e: e.dma_start(out=out_ap, in_=in_ap, **kw), pw=[r_c])

        ident = sbt(G, "ident", [128, 128], F32)
        identb = sbt(G, "identb", [128, 128], BF16)
        onesb = sbt(G, "onesb", [128, 128], BF16)
        onesf = sbt(G, "onesf", [128, 128], F32)
        lnG = [sbt(G, f"lnG{k}", [128, D], F32) for k in range(3)]
        lnB = [sbt(G, f"lnB{k}", [128, D], F32) for k in range(3)]
        posk = sbt(G, "posk", [128, NT, 4], I32); r_posk = [Reg() for _ in range(NT)]
        gk = sbt(G, "gk", [128, NT, 4], F32); r_gk = [Reg() for _ in range(NT)]
        pmask = sbt(G, "pmaskt", [128, 1], F32)
        stat = sbt(G, "stat", [128, 16], F32); r_stat = Reg()
        epsT = sbt(G, "epsT", [128, 4], F32)
        S.op('dve', lambda e: e.memset(epsT[:, 0:1], 1e-6), pw=[r_c])
        S.op('dve', lambda e: e.memset(epsT[:, 1:2], 1e-5), pw=[r_c])
        S.op('dve', lambda e: e.memset(epsT[:, 2:3], 1.0), pw=[r_c])
        S.op('dve', lambda e: e.memset(epsT[:, 3:4], 0.0), pw=[r_c])
        cload('sp', ident[:], c_ident)
        cload('pool', identb[:], c_ident)
        cload('sp', pmask[:], pmask_d)
        S.op('dve', lambda e: e.memset(onesf[:], 1.0), pw=[r_c])
        S.op('dve', lambda e: e.memset(onesb[:], 1.0), pw=[r_c])
        for k in range(3):
            cload('sp', lnG[k][:], ln_g[k].partition_broadcast(128))
            cload('sp', lnB[k][:], ln_b[k].partition_broadcast(128))


        def run_staggered(gen_fn, n, lag):
            active = []; steps = {}; nxt = 0
            while nxt < n or active:
                if nxt < n and len(active) < 2 and (not active or steps[id(active[-1])] >= lag):
                    g_ = gen_fn(nxt); active.append(g_); steps[id(g_)] = 0; nxt += 1
                for g_ in list(active):
                    try:
                        next(g_); steps[id(g_)] += 1
                    except StopIteration:
                        active.remove(g_)

        def layer_norm(src, src_reg, k, dst, dst_reg):
            for hh in range(2):
                S.op('dve', lambda e: e.bn_stats(out=stat[:, 6 * hh:6 * hh + 6], in_=src[:, 512 * hh:512 * hh + 512]),
                     reads=[src_reg], writes=[r_stat] if hh == 0 else [], pw=[] if hh == 0 else [r_stat])
            S.op('dve', lambda e: e.bn_aggr(out=stat[:, 12:14], in_=stat[:, 0:12]), reads=[r_stat], pw=[r_stat])
            S.op('act', lambda e: e.activation(out=stat[:, 15:16], in_=stat[:, 13:14], func=AF.Sqrt, bias=epsT[:, 1:2]), reads=[r_stat] + RC, pw=[r_stat])
            S.op('dve', lambda e: e.reciprocal(out=stat[:, 14:15], in_=stat[:, 15:16]), reads=[r_stat], pw=[r_stat])
            S.op('dve', lambda e: e.tensor_scalar(out=src[:], in0=src[:], scalar1=stat[:, 12:13], scalar2=stat[:, 14:15],
                                                  op0=ALU.subtract, op1=ALU.mult), reads=[src_reg, r_stat], writes=[src_reg])
            S.op('dve', lambda e: e.tensor_tensor(out=src[:], in0=src[:], in1=lnG[k][:], op=ALU.mult),
                 reads=[src_reg] + RC, writes=[src_reg])
            S.op('dve', lambda e: e.tensor_tensor(out=dst[:], in0=src[:], in1=lnB[k][:], op=ALU.add),
                 reads=[src_reg] + RC, writes=[dst_reg])

        with ExitStack() as P1:
            wb = sbt(P1, "wb", [128, 8, DINP], BF16); r_wb = Reg()
            for dc in range(8):
                S.dma('pool', lambda e: e.dma_start(out=wb[:, dc, :], in_=w_in[dc * 128:(dc + 1) * 128, :]), pw=[r_wb])
            trichi = sbt(P1, "trichi", [128, 130], F32); cload('sp', trichi[:], c_trichi)
            blk = sbt(P1, "blk", [128, 128], F32); cload('sp', blk[:], c_blk)
            negt = sbt(P1, "negt", [128, 128], F32); cload('sp', negt[:], c_negt)
            smask = sbt(P1, "smask", [128, 128], F32); cload('sp', smask[:], c_smask)
            cw = sbt(P1, "cw", [128, 12, 4], F32)
            cload('sp', cw[:], conv_w)
            gpar = sbt(P1, "gpar", [128, 12], F32); r_gpar = Reg()
            cload('sp', gpar[:, 0:4], a_log.partition_broadcast(128))
            cload('sp', gpar[:, 4:8], dt_bias.partition_broadcast(128))
            S.op('act', lambda e: e.activation(out=gpar[:, 8:12], in_=gpar[:, 0:4], func=AF.Exp), reads=RC, writes=[r_gpar])
            S.op('dve', lambda e: e.tensor_scalar(out=gpar[:, 8:12], in0=gpar[:, 8:12], scalar1=-1.0, scalar2=None, op0=ALU.mult),
                 reads=[r_gpar], writes=[r_gpar])
            gnwb = sbt(P1, "gnwb", [128, 128], F32); cload('sp', gnwb[:], gnw.partition_broadcast(128))

            xb = [sbt(P1, f"xb{i}", [128, D], BF16) for i in range(2)]; r_xb = [Reg(), Reg()]
            xT = sbt(P1, "xT", [128, 8, 128], BF16); r_xT = Reg()
            cb = sbt(P1, "cb", [128, 12, 131], F32); r_cb = Reg()
            cacc = sbt(P1, "cacc", [128, 12, 128], F32); r_cacc = Reg()
            qkv = sbt(P1, "qkv", [128, 12, 128], F32); r_qkv = Reg()
            sq = sbt(P1, "sq", [128, 8, 128], BF16); r_sq = Reg()
            rn = sbt(P1, "rn", [128, 8, 128], F32); r_rn = Reg()
            qkn_2 = [sbt(P1, f"qkn{i}", [128, 8, 128], BF16) for i in range(2)]; r_qkn_2 = [Reg(), Reg()]
            vsb = sbt(P1, "vsb", [128, 4, 128], BF16); r_vsb = Reg()
            ktok_2 = [sbt(P1, f"ktok{i}", [128, 4, 2, 128], BF16) for i in range(2)]; r_ktok_2 = [Reg(), Reg()]
            vtok_2 = [sbt(P1, f"vtok{i}", [128, 4, 128], BF16) for i in range(2)]; r_vtok_2 = [Reg(), Reg()]
            gt_2 = [sbt(P1, f"gt{i}", [128, 32], F32) for i in range(2)]; r_gt_2 = [Reg(), Reg()]
            dqk = sbt(P1, "dqk", [128, 8, 128], BF16); r_dqk = Reg()
            dvz = sbt(P1, "dvz", [128, 4, VW], BF16); r_dvz = Reg()
            zs_2 = [sbt(P1, f"zs{i}", [128, 512], F32) for i in range(2)]; r_zs_2 = [Reg(), Reg()]
            gbc = [sbt(P1, f"gbc{h}", [128, 2, 128], F32) for h in range(4)]; r_gbc = [Reg() for _ in range(4)]
            dect = [sbt(P1, f"dect{h}", [128, 128], F32) for h in range(4)]; r_dect = [Reg() for _ in range(4)]
            decs = [sbt(P1, f"decs{h}", [128, 128], F32) for h in range(4)]; r_decs = [Reg() for _ in range(4)]
            brow = [sbt(P1, f"brow{h}", [128, 128], F32) for h in range(4)]; r_brow = [Reg() for _ in range(4)]
            egrow = [sbt(P1, f"egrow{h}", [128, 128], F32) for h in range(4)]; r_egrow = [Reg() for _ in range(4)]
            glt = [sbt(P1, f"glt{h}", [128, 2], F32) for h in range(4)]; r_glt = [Reg() for _ in range(4)]
            Xm = [[sbt(P1, f"Xm{h}{i}", [128, 128], F32) for i in range(2)] for h in range(4)]; r_Xm = [[Reg(), Reg()] for _ in range(4)]
            Ym = [[sbt(P1, f"Ym{h}{i}", [128, 128], F32) for i in range(2)] for h in range(4)]; r_Ym = [[Reg(), Reg()] for _ in range(4)]
            Tt = [[sbt(P1, f"Tt{h}{i}", [128, 128], F32) for i in range(2)] for h in range(4)]; r_Tt = [[Reg(), Reg()] for _ in range(4)]
            Ttb = [sbt(P1, f"Ttb{h}", [128, 128], BF16) for h in range(4)]; r_Ttb = [Reg() for _ in range(4)]
            intT = [sbt(P1, f"intT{h}", [128, 128], BF16) for h in range(4)]; r_intT = [Reg() for _ in range(4)]
            usb = [sbt(P1, f"usb{h}", [128, 128], F32) for h in range(4)]; r_usb = [Reg() for _ in range(4)]
            wT = [sbt(P1, f"wT{h}", [128, 128], BF16) for h in range(4)]; r_wT = [Reg() for _ in range(4)]
            qd = [sbt(P1, f"qd{h}", [128, 2, 128], BF16) for h in range(4)]; r_qd = [Reg() for _ in range(4)]
            vnp = [sbt(P1, f"vnp{h}", [128, 2, 128], BF16) for h in range(4)]; r_vnp = [Reg() for _ in range(4)]
            Sf = [sbt(P1, f"Sf{h}", [128, 128], F32) for h in range(4)]; r_Sf = [Reg() for _ in range(4)]
            Sb = [sbt(P1, f"Sb{h}", [128, 2, 128], BF16) for h in range(4)]; r_Sb = [[Reg(), Reg()] for _ in range(4)]
            osb = [sbt(P1, f"osb{h}", [128, 128], F32) for h in range(4)]; r_osb = [Reg() for _ in range(4)]
            osq = [sbt(P1, f"osq{h}", [128, 128], F32) for h in range(4)]; r_osq = [Reg() for _ in range(4)]
            ost = [sbt(P1, f"ost{h}", [128, 4], F32) for h in range(4)]; r_ost = [Reg() for _ in range(4)]
            hg_2 = [sbt(P1, f"hg{i}", [128, 512], BF16) for i in range(2)]; r_hg_2 = [Reg(), Reg()]

            pT = pst(P1, "pT", [128, 1024], BF16); r_pT = Reg(True)
            pT2 = pst(P1, "pT2", [128, 1024], BF16); r_pT2 = Reg(True)
            pF1 = pst(P1, "pF", [128, 512], F32); pF = [pF1, pF1]; _rpf = Reg(True); r_pF = [_rpf, _rpf]
            pM = pst(P1, "pM", [128, 512], F32); r_pM = Reg(True)
            pH = [pst(P1, f"pH{i}", [128, 512], F32) for i in range(4)]
            r_pH = [Reg(True) for _ in range(4)]
            SLOTA = [0, 128, 258]
            SLOTB = [0, 128, 256, 384]

            def pg(b, s, n=128):
                o = SLOTB[s] if b == 2 else SLOTA[s]
                return pG[b][:, o:o + n]

            S.op('dve', lambda e: e.memset(dvz[:].rearrange("p a b -> p (a b)"), 1.0), writes=[r_dvz])
            S.op('dve', lambda e: e.memset(cb[:].rearrange("p a b -> p (a b)"), 0.0), writes=[r_cb])
            for h in range(4):
                S.op('dve', lambda e: e.memset(qd[h][:].rearrange("p a b -> p (a b)"), 0.0), writes=[r_qd[h]])
                S.op('dve', lambda e: e.memset(vnp[h][:].rearrange("p a b -> p (a b)"), 0.0), writes=[r_vnp[h]])
            for h in range(4):
                S.op('dve', lambda e: e.memset(Sf[h][:], 0.0), writes=[r_Sf[h]])
                S.op('dve', lambda e: e.memset(Sb[h][:].rearrange("p a b -> p (a b)"), 0.0), writes=r_Sb[h])

            def load_x(t):
                S.dma('pool', lambda e: e.dma_start(out=xb[t % 2][:], in_=x_all[t * 128:(t + 1) * 128, :]), writes=[r_xb[t % 2]])

            load_x(0)
            fm_groups = [(C_GQ, 0), (C_GK, 1), (C_GV, 2), (C_DQ, 3), (C_DK, 4)]
            def front_gen(t):
                gt, r_gt = gt_2[t % 2], r_gt_2[t % 2]; qkn, r_qkn = qkn_2[t % 2], r_qkn_2[t % 2]; ktok, r_ktok = ktok_2[t % 2], r_ktok_2[t % 2]
                vtok, r_vtok = vtok_2[t % 2], r_vtok_2[t % 2]; zs, r_zs = zs_2[t % 2], r_zs_2[t % 2]
                own = t >= NT
                ti = t - NT
                if t + 1 < NTT:
                    load_x(t + 1)
                for dc in range(8):
                    S.op('pe', lambda e: e.transpose(out=pT[:, dc * 128:(dc + 1) * 128], in_=xb[t % 2][:, dc * 128:(dc + 1) * 128], identity=identb[:]),
                         reads=[r_xb[t % 2]] + RC, writes=[r_pT])
                S.op('act', lambda e: e.activation(out=xT[:, 0:4, :], in_=R3(pT[:, 0:512]), func=AF.Copy), reads=[r_pT], pw=[r_xT])
                S.op('dve', lambda e: e.tensor_copy(out=xT[:, 4:8, :], in_=R3(pT[:, 512:1024])), reads=[r_pT], pw=[r_xT])
                yield
                gi = 0
                for (c0, gidx) in fm_groups:
                    if gidx == 0 and not (own or t == NT - 1):
                        continue
                    if gidx == 3 and not own:
                        continue
                    pf = pF[gi % 2]; rpf = r_pF[gi % 2]; gi += 1
                    for h in range(4):
                        for dc in range(8):
                            S.op('pe', lambda e: e.matmul(pf[:, h * 128:(h + 1) * 128], lhsT=wb[:, dc, c0 + h * 128:c0 + (h + 1) * 128],
                                                          rhs=xT[:, dc, :], start=(dc == 0), stop=(dc == 7)),
                                 reads=[r_wb, r_xT], writes=[rpf])
                    if gidx < 3:
                        S.op('act', lambda e: e.activation(out=cb[:, gidx * 4:gidx * 4 + 4, 3:131], in_=R3(pf[:]), func=AF.Copy),
                             reads=[rpf], pw=[r_cb])
                        yield
                    else:
                        o0 = 0 if gidx == 3 else 4
                        S.op('act', lambda e: e.activation(out=dqk[:, o0:o0 + 4, :], in_=R3(pf[:]), func=AF.Copy), reads=[rpf], pw=[r_dqk])
                        yield
                for dc in range(8):
                    S.op('pe', lambda e: e.matmul(pM[:, :], lhsT=xT[:, dc, :], rhs=wb[:, dc, C_DV:C_DV + 512], start=(dc == 0), stop=(dc == 7)),
                         reads=[r_wb, r_xT], writes=[r_pM])
                S.op('dve', lambda e: e.tensor_copy(out=dvz[:, :, 0:128], in_=R3(pM[:])), reads=[r_pM], pw=[r_dvz])
                yield
                S.dma('sp', lambda e: e.dma_start(out=DVs[t * 128:(t + 1) * 128, :], in_=dvz[:].rearrange("p a b -> p (a b)")), reads=[r_dvz], pw=[r_dv])
                for h in range(4):
                    S.dma('sp', lambda e: e.dma_start(out=DKs[h, :, t * 128:(t + 1) * 128], in_=dqk[:, 4 + h, :]), reads=[r_dqk], pw=[r_dk])
                    if own:
                        S.dma('sp', lambda e: e.dma_start(out=DQs[h, :, ti * 128:(ti + 1) * 128], in_=dqk[:, h, :]), reads=[r_dqk], pw=[r_dq])
                if own:
                    for dc in range(8):
                        S.op('pe', lambda e: e.matmul(pM[:, :], lhsT=xT[:, dc, :], rhs=wb[:, dc, C_Z:C_Z + 512], start=(dc == 0), stop=(dc == 7)),
                             reads=[r_wb, r_xT], writes=[r_pM])
                    S.op('act', lambda e: e.activation(out=zs[:], in_=pM[:], func=AF.Silu), reads=[r_pM], writes=[r_zs])
                    yield
                for dc in range(8):
                    S.op('pe', lambda e: e.matmul(pM[:, 0:8], lhsT=xT[:, dc, :], rhs=wb[:, dc, C_A:C_A + 8], start=(dc == 0), stop=(dc == 7)),
                         reads=[r_wb, r_xT], writes=[r_pM])
                S.op('dve', lambda e: e.tensor_tensor(out=gt[:, 0:4], in0=pM[:, 0:4], in1=gpar[:, 4:8], op=ALU.add),
                     reads=[r_pM, r_gpar] + RC, writes=[r_gt])
                yield
                S.op('act', lambda e: e.activation(out=gt[:, 0:4], in_=gt[:, 0:4], func=AF.Exp), reads=[r_gt], writes=[r_gt])
                S.op('act', lambda e: e.activation(out=gt[:, 0:4], in_=gt[:, 0:4], func=AF.Ln, bias=epsT[:, 2:3]), reads=[r_gt] + RC, writes=[r_gt])
                S.op('dve', lambda e: e.tensor_tensor(out=gt[:, 0:4], in0=gt[:, 0:4], in1=gpar[:, 8:12], op=ALU.mult),
                     reads=[r_gt, r_gpar], writes=[r_gt])
                S.op('act', lambda e: e.activation(out=gt[:, 4:8], in_=pM[:, 4:8], func=AF.Sigmoid), reads=[r_pM, r_gt], writes=[r_gt])
                S.op('dve', lambda e: e.tensor_scalar(out=gt[:, 28:32], in0=gt[:, 4:8], scalar1=-1.0, scalar2=None, op0=ALU.mult),
                     reads=[r_gt], writes=[r_gt])
                yield
                S.op('pe', lambda e: e.matmul(pM[:, 16:20], lhsT=trichi[:, 0:128], rhs=gt[:, 0:4], start=True, stop=True),
                     reads=[r_gt] + RC, writes=[r_pM])
                S.op('pe', lambda e: e.matmul(pM[:, 20:24], lhsT=blk[:], rhs=gt[:, 0:4], start=True, stop=True),
                     reads=[r_gt] + RC, pw=[r_pM])
                S.op('dve', lambda e: e.tensor_copy(out=gt[:, 8:16], in_=pM[:, 16:24]), reads=[r_pM, r_gt], writes=[r_gt])
                yield
                S.op('act', lambda e: e.activation(out=gt[:, 16:20], in_=gt[:, 8:12], func=AF.Exp), reads=[r_gt], writes=[r_gt])
                S.op('dve', lambda e: e.tensor_tensor(out=gt[:, 20:24], in0=gt[:, 12:16], in1=gt[:, 8:12], op=ALU.subtract), reads=[r_gt], writes=[r_gt])
                S.op('act', lambda e: e.activation(out=gt[:, 20:24], in_=gt[:, 20:24], func=AF.Exp), reads=[r_gt], writes=[r_gt])
                S.op('dve', lambda e: e.tensor_scalar(out=gt[:, 24:28], in0=gt[:, 8:12], scalar1=-1.0, scalar2=None, op0=ALU.mult), reads=[r_gt], writes=[r_gt])
                yield
                c_lo = 0 if own else 4
                ncn = 12 - c_lo
                for k in range(4):
                    in1 = cw[:, c_lo:12, k:k + 1].to_broadcast([128, ncn, 128])
                    if k == 0:
                        S.op('dve', lambda e: e.tensor_tensor(out=cacc[:, c_lo:12, :], in0=cb[:, c_lo:12, 0:128], in1=in1, op=ALU.mult),
                             reads=[r_cb] + RC, writes=[r_cacc])
                    else:
                        S.op('pool', lambda e: e.tensor_tensor(out=qkv[:, c_lo:12, :], in0=cb[:, c_lo:12, k:k + 128], in1=in1, op=ALU.mult),
                             reads=[r_cb] + RC, writes=[r_qkv])
                        S.op('dve', lambda e: e.tensor_tensor(out=cacc[:, c_lo:12, :], in0=cacc[:, c_lo:12, :], in1=qkv[:, c_lo:12, :], op=ALU.add),
                             reads=[r_cacc, r_qkv], writes=[r_cacc])
                    yield
                S.op('pool', lambda e: e.tensor_copy(out=cb[:, :, 0:3], in_=cb[:, :, 128:131]), reads=[r_cb], pw=[r_cb])
                S.op('act', lambda e: e.activation(out=qkv[:, c_lo:12, :], in_=cacc[:, c_lo:12, :], func=AF.Silu), reads=[r_cacc], writes=[r_qkv])
                yield
                S.op('dve', lambda e: e.tensor_tensor(out=sq[:, c_lo:8, :], in0=qkv[:, c_lo:8, :], in1=qkv[:, c_lo:8, :], op=ALU.mult),
                     reads=[r_qkv], writes=[r_sq])
                yield
                for half in ([0, 1] if own else [1]):
                    pf = pF[half]; rpf = r_pF[half]
                    S.op('pe', lambda e: e.matmul(pf[:, :], lhsT=onesb[:], rhs=sq[:, half * 4:half * 4 + 4, :], start=True, stop=True),
                         reads=[r_sq] + RC, writes=[rpf])
                    S.op('act', lambda e: e.activation(out=rn[:, half * 4:half * 4 + 4, :], in_=R3(pf[:]), func=AF.Sqrt, bias=epsT[:, 0:1]),
                         reads=[rpf] + RC, pw=[r_rn])
                    S.op('dve', lambda e: e.reciprocal(out=rn[:, half * 4:half * 4 + 4, :], in_=rn[:, half * 4:half * 4 + 4, :]),
                         reads=[r_rn], pw=[r_rn])
                    yield
                if own:
                    S.op('dve', lambda e: e.scalar_tensor_tensor(out=qkn[:, 0:4, :], in0=qkv[:, 0:4, :], scalar=128.0 ** -0.5, in1=rn[:, 0:4, :],
                                                                 op0=ALU.mult, op1=ALU.mult), reads=[r_qkv, r_rn], pw=[r_qkn])
                S.op('pool', lambda e: e.tensor_tensor(out=qkn[:, 4:8, :], in0=qkv[:, 4:8, :], in1=rn[:, 4:8, :], op=ALU.mult),
                     reads=[r_qkv, r_rn], pw=[r_qkn])
                S.op('pool', lambda e: e.tensor_copy(out=vsb[:], in_=qkv[:, 8:12, :]), reads=[r_qkv], writes=[r_vsb])
                yield
                for h in range(4):
                    S.op('pe', lambda e: e.transpose(out=pT2[:, h * 128:(h + 1) * 128], in_=qkn[:, 4 + h, :], identity=identb[:]),
                         reads=[r_qkn] + RC, writes=[r_pT2])
                    S.op('pe', lambda e: e.transpose(out=pT2[:, 512 + h * 128:512 + (h + 1) * 128], in_=vsb[:, h, :], identity=identb[:]),
                         reads=[r_vsb] + RC, writes=[r_pT2])
                for h in range(4):
                    S.op('dve', lambda e: e.tensor_scalar(out=ktok[:, h, 0, :], in0=pT2[:, h * 128:(h + 1) * 128], scalar1=gt[:, 20 + h:21 + h], scalar2=None, op0=ALU.mult),
                         reads=[r_pT2, r_gt], pw=[r_ktok])
                    S.op('dve', lambda e: e.tensor_scalar(out=ktok[:, h, 1, :], in0=pT2[:, h * 128:(h + 1) * 128], scalar1=gt[:, 16 + h:17 + h], scalar2=None, op0=ALU.mult),
                         reads=[r_pT2, r_gt], pw=[r_ktok])
                S.op('act', lambda e: e.activation(out=vtok[:], in_=R3(pT2[:, 512:1024]), func=AF.Copy), reads=[r_pT2], writes=[r_vtok])
                yield

            def head_gen(t, h):
                own = t >= NT
                gt, r_gt = gt_2[t % 2], r_gt_2[t % 2]; qkn, r_qkn = qkn_2[t % 2], r_qkn_2[t % 2]; ktok, r_ktok = ktok_2[t % 2], r_ktok_2[t % 2]
                vtok, r_vtok = vtok_2[t % 2], r_vtok_2[t % 2]; zs, r_zs = zs_2[t % 2], r_zs_2[t % 2]; hg, r_hg = hg_2[t % 2], r_hg_2[t % 2]
                bank = pH[h]; rb = r_pH[h]
                A = bank[:, 0:128]; B = bank[:, 128:258]; Bm = bank[:, 128:256]; Bgl = bank[:, 256:258]; C = bank[:, 258:386]
                S.op('dve', lambda e: e.tensor_scalar(out=gbc[h][:, 0, :], in0=onesf[:], scalar1=gt[:, h:h + 1], scalar2=None, op0=ALU.mult),
                     reads=[r_gt] + RC, pw=[r_gbc[h]])
                S.op('dve', lambda e: e.tensor_scalar(out=gbc[h][:, 1, :], in0=onesf[:], scalar1=gt[:, 4 + h:5 + h], scalar2=None, op0=ALU.mult),
                     reads=[r_gt] + RC, pw=[r_gbc[h]])
                yield
                S.op('pe', lambda e: e.matmul(A, lhsT=gbc[h][:, 0, :], rhs=trichi[:, 0:128], start=True, stop=False), reads=[r_gbc[h]] + RC, writes=[rb])
                S.op('pe', lambda e: e.matmul(A, lhsT=ident[:], rhs=negt[:], start=False, stop=True), reads=RC, writes=[rb])
                S.op('pe', lambda e: e.matmul(B, lhsT=gbc[h][:, 0, :], rhs=trichi[:, 0:130], start=True, stop=True), reads=[r_gbc[h]] + RC, writes=[rb])
                S.op('pe', lambda e: e.matmul(C, lhsT=gbc[h][:, 1, :], rhs=ident[:], start=True, stop=True), reads=[r_gbc[h]] + RC, writes=[rb])
                yield
                S.op('act', lambda e: e.activation(out=dect[h][:], in_=A, func=AF.Exp, bias=gt[:, 24 + h:25 + h]), reads=[rb, r_gt], writes=[r_dect[h]])
                S.op('act', lambda e: e.activation(out=glt[h][:], in_=Bgl, func=AF.Exp), reads=[rb], writes=[r_glt[h]])
                if own:
                    S.op('act', lambda e: e.activation(out=egrow[h][:], in_=Bm, func=AF.Exp), reads=[rb], writes=[r_egrow[h]])
                S.op('act', lambda e: e.activation(out=brow[h][:], in_=C, func=AF.Copy), reads=[rb], writes=[r_brow[h]])
                yield
                S.op('pool', lambda e: e.tensor_tensor(out=decs[h][:], in0=dect[h][:], in1=smask[:], op=ALU.mult), reads=[r_dect[h]] + RC, writes=[r_decs[h]])
                S.op('pe', lambda e: e.matmul(A, lhsT=qkn[:, 4 + h, :], rhs=qkn[:, 4 + h, :], start=True, stop=True), reads=[r_qkn], writes=[rb])
                if own:
                    S.op('pe', lambda e: e.matmul(Bm, lhsT=qkn[:, 4 + h, :], rhs=qkn[:, h, :], start=True, stop=True), reads=[r_qkn], writes=[rb])
                yield
                S.op('dve', lambda e: e.scalar_tensor_tensor(out=Ym[h][0][:], in0=A, scalar=gt[:, 28 + h:29 + h], in1=decs[h][:],
                                                             op0=ALU.mult, op1=ALU.mult), reads=[rb, r_gt, r_decs[h]], writes=[r_Ym[h][0]])
                if own:
                    S.op('dve', lambda e: e.tensor_tensor(out=intT[h][:], in0=Bm, in1=dect[h][:], op=ALU.mult), reads=[rb, r_dect[h]], writes=[r_intT[h]])
                    for c in range(2):
                        S.op('pool', lambda e: e.tensor_tensor(out=qd[h][:, c, c * 64:(c + 1) * 64], in0=qkn[:, h, c * 64:(c + 1) * 64],
                                                               in1=egrow[h][:, c * 64:(c + 1) * 64], op=ALU.mult), reads=[r_qkn, r_egrow[h]], pw=[r_qd[h]])
                yield
                S.op('pe', lambda e: e.transpose(out=C, in_=Ym[h][0][:], identity=ident[:]), reads=[r_Ym[h][0]] + RC, writes=[rb])
                S.op('pool', lambda e: e.tensor_tensor(out=Tt[h][0][:], in0=Ym[h][0][:], in1=ident[:], op=ALU.add), reads=[r_Ym[h][0]] + RC, writes=[r_Tt[h][0]])
                yield
                S.op('act', lambda e: e.activation(out=Xm[h][0][:], in_=C, func=AF.Copy), reads=[rb], writes=[r_Xm[h][0]])
                yield
                S.op('pe', lambda e: e.matmul(A, lhsT=Ym[h][0][:], rhs=Xm[h][0][:], start=True, stop=True), reads=[r_Ym[h][0], r_Xm[h][0]], writes=[rb])
                S.op('pe', lambda e: e.matmul(Bm, lhsT=Xm[h][0][:], rhs=Ym[h][0][:], start=True, stop=True), reads=[r_Ym[h][0], r_Xm[h][0]], writes=[rb])
                yield
                for k in range(5):
                    a, b = k % 2, (k + 1) % 2
                    if k > 0:
                        S.op('dve', lambda e: e.tensor_tensor(out=Tt[h][a][:], in0=C, in1=Tt[h][b][:], op=ALU.add), reads=[rb, r_Tt[h][b]], writes=[r_Tt[h][a]])
                    S.op('act', lambda e: e.activation(out=Xm[h][b][:], in_=A, func=AF.Copy), reads=[rb], writes=[r_Xm[h][b]])
                    if k < 4:
                        S.op('dve', lambda e: e.tensor_copy(out=Ym[h][b][:], in_=Bm), reads=[rb], writes=[r_Ym[h][b]])
                    yield
                    S.op('pe', lambda e: e.matmul(C, lhsT=Xm[h][b][:], rhs=Tt[h][a][:], start=True, stop=True), reads=[r_Xm[h][b], r_Tt[h][a]], writes=[rb])
                    if k < 4:
                        S.op('pe', lambda e: e.matmul(A, lhsT=Ym[h][b][:], rhs=Xm[h][b][:], start=True, stop=True), reads=[r_Ym[h][b], r_Xm[h][b]], writes=[rb])
                    if k < 3:
                        S.op('pe', lambda e: e.matmul(Bm, lhsT=Xm[h][b][:], rhs=Ym[h][b][:], start=True, stop=True), reads=[r_Ym[h][b], r_Xm[h][b]], writes=[rb])
                    yield
                S.op('dve', lambda e: e.tensor_tensor(out=Tt[h][1][:], in0=C, in1=Tt[h][0][:], op=ALU.add), reads=[rb, r_Tt[h][0]], writes=[r_Tt[h][1]])
                S.op('dve', lambda e: e.tensor_tensor(out=Ttb[h][:], in0=brow[h][:], in1=Tt[h][1][:], op=ALU.mult), reads=[r_brow[h], r_Tt[h][1]], writes=[r_Ttb[h]])
                yield
                S.op('pe', lambda e: e.matmul(A, lhsT=Ttb[h][:], rhs=vtok[:, h, :], start=True, stop=True), reads=[r_Ttb[h], r_vtok], writes=[rb])
                S.op('pe', lambda e: e.matmul(Bm, lhsT=ktok[:, h, 1, :], rhs=Ttb[h][:], start=True, stop=True), reads=[r_Ttb[h], r_ktok], writes=[rb])
                yield
                S.op('act', lambda e: e.activation(out=usb[h][:], in_=A, func=AF.Copy), reads=[rb], writes=[r_usb[h]])
                S.op('dve', lambda e: e.tensor_copy(out=wT[h][:], in_=Bm), reads=[rb], writes=[r_wT[h]])
                yield
                for c in range(2):
                    lo, hi = c * 64, (c + 1) * 64
                    S.op('pe', lambda e: e.matmul(A, lhsT=wT[h][:], rhs=Sb[h][:, c, :], start=True, stop=True), reads=[r_wT[h], r_Sb[h][c]], writes=[rb])
                    yield
                    S.op('dve', lambda e: e.tensor_tensor(out=vnp[h][lo:hi, c, :], in0=usb[h][lo:hi, :], in1=bank[lo:hi, 0:128], op=ALU.subtract),
                         reads=[r_usb[h], rb], pw=[r_vnp[h]])
                    yield
                    S.op('pe', lambda e: e.matmul(Bm, lhsT=ktok[:, h, 0, :], rhs=vnp[h][:, c, :], start=True, stop=True), reads=[r_ktok, r_vnp[h]], writes=[rb])
                    if own and c == 1:
                        S.op('pe', lambda e: e.matmul(C, lhsT=qd[h][:, 0, :], rhs=Sb[h][:, 0, :], start=True, stop=False), reads=[r_qd[h], r_Sb[h][0]], writes=[rb])
                        S.op('pe', lambda e: e.matmul(C, lhsT=qd[h][:, 1, :], rhs=Sb[h][:, 1, :], start=False, stop=False), reads=[r_qd[h], r_Sb[h][1]], writes=[rb])
                        S.op('pe', lambda e: e.matmul(C, lhsT=intT[h][:], rhs=vnp[h][:, 0, :], start=False, stop=False), reads=[r_intT[h], r_vnp[h]], writes=[rb])
                        S.op('pe', lambda e: e.matmul(C, lhsT=intT[h][:], rhs=vnp[h][:, 1, :], start=False, stop=True), reads=[r_intT[h], r_vnp[h]], writes=[rb])
                    yield
                    S.op('dve', lambda e: e.scalar_tensor_tensor(out=Sf[h][:], in0=Sf[h][:], scalar=glt[h][:, c:c + 1], in1=Bm,
                                                                 op0=ALU.mult, op1=ALU.add), reads=[r_Sf[h], r_glt[h], rb], writes=[r_Sf[h]])
                    S.op('act', lambda e: e.activation(out=Sb[h][:, 1 - c, :], in_=Sf[h][:], func=AF.Copy), reads=[r_Sf[h]], writes=[r_Sb[h][1 - c]])
                    yield
                if own:
                    S.op('act', lambda e: e.activation(out=osb[h][:], in_=C, func=AF.Copy), reads=[rb], writes=[r_osb[h]])
                    S.op('act', lambda e: e.activation(out=osq[h][:], in_=osb[h][:], func=AF.Square, accum_out=ost[h][:, 0:1]),
                         reads=[r_osb[h]], writes=[r_osq[h], r_ost[h]])
                    S.op('act', lambda e: e.activation(out=ost[h][:, 1:2], in_=ost[h][:, 0:1], func=AF.Sqrt, bias=epsT[:, 0:1], scale=1.0 / 128.0),
                         reads=[r_ost[h]] + RC, writes=[r_ost[h]])
                    yield
                    S.op('dve', lambda e: e.reciprocal(out=ost[h][:, 2:3], in_=ost[h][:, 1:2]), reads=[r_ost[h]], writes=[r_ost[h]])
                    S.op('dve', lambda e: e.scalar_tensor_tensor(out=osq[h][:], in0=osb[h][:], scalar=ost[h][:, 2:3], in1=gnwb[:], op0=ALU.mult, op1=ALU.mult),
                         reads=[r_osb[h], r_ost[h]] + RC, writes=[r_osq[h]])
                    yield
                    S.op('pool', lambda e: e.tensor_tensor(out=hg[:, h * 128:(h + 1) * 128], in0=osq[h][:], in1=zs[:, h * 128:(h + 1) * 128], op=ALU.mult),
                         reads=[r_osq[h], r_zs], pw=[r_hg])


            for t in range(NTT + 1):
                gens = []
                if t < NTT:
                    gens.append(front_gen(t))
                if t >= 1:
                    gens += [head_gen(t - 1, h) for h in range(4)]
                while gens:
                    for g_ in list(gens):
                        try:
                            next(g_)
                        except StopIteration:
                            gens.remove(g_)
                if t >= 1 and (t - 1) >= NT:
                    ti = t - 1 - NT
                    S.dma('sp', lambda e: e.dma_start(out=HDs[ti * 128:(ti + 1) * 128, 0:512], in_=hg_2[(t - 1) % 2][:]), reads=[r_hg_2[(t - 1) % 2]], pw=[r_hd[ti]])
            S.barrier()
        if stop_after <= 1:
            return nc, S

        lamT = sbt(G, "lamT", [128, 8], F32); r_lam = Reg()
        with ExitStack() as P2:
            biasN = sbt(P2, "biasN", [128, 8, 128], F32); r_bN = [Reg() for _ in range(8)]
            biasB = sbt(P2, "biasB", [128, 8, 128], BF16); r_bB = Reg()
            rbb = sbt(P2, "rbb", [128, 128], F32); cload('sp', rbb[:], rel_bias.partition_broadcast(128))
            bfp = sbt(P2, "bfp", [128, 8], F32); r_bfp = Reg()
            dnwb = sbt(P2, "dnwb", [128, 128], F32); cload('sp', dnwb[:], dnw.partition_broadcast(128)); r_dnwb = Reg()
            S.op('dve', lambda e: e.tensor_scalar(out=dnwb[:], in0=dnwb[:], scalar1=1.0 - LAM_INIT, scalar2=None, op0=ALU.mult), reads=RC, writes=[r_dnwb])
            with ExitStack() as P2a:
                t5 = sbt(P2a, "t5", [128, 64, 128], F32); cload('sp', t5[:], c_t5oh)
                negd = sbt(P2a, "negd", [128, 128], F32); cload('sp', negd[:], c_negd)
                lq = sbt(P2a, "lq", [128, 4, 64], F32)
                for k in range(4):
                    cload('sp', lq[:, k, :], lqk[k].partition_broadcast(128))
                for ty in range(2):
                    for h in range(4):
                        dst = biasN[:, ty * 4 + h, :]
                        rb_ = r_bN[ty * 4 + h]
                        S.op('dve', lambda e: e.tensor_scalar(out=dst, in0=t5[:, ty * 32, :], scalar1=rbb[:, h:h + 1], scalar2=None, op0=ALU.mult),
                             reads=RC, writes=[rb_])
                        for b in range(1, 32):
                            S.op('dve', lambda e: e.scalar_tensor_tensor(out=dst, in0=t5[:, ty * 32 + b, :], scalar=rbb[:, b * 4 + h:b * 4 + h + 1], in1=dst,
                                                                         op0=ALU.mult, op1=ALU.add), reads=RC + [rb_], writes=[rb_])
                        if ty == 0:
                            S.op('dve', lambda e: e.tensor_tensor(out=dst, in0=dst, in1=negd[:], op=ALU.add), reads=RC + [rb_], writes=[rb_])
                for ty in range(2):
                    for h in range(4):
                        S.op('dve', lambda e: e.tensor_scalar(out=biasB[:, ty * 4 + h, :], in0=biasN[:, ty * 4 + h, :], scalar1=rbb[:, 124 + h:125 + h], scalar2=8.0,
                                                              op0=ALU.subtract, op1=ALU.mult), reads=RC + [r_bN[ty * 4 + h]], pw=[r_bB])
                S.op('dve', lambda e: e.tensor_copy(out=bfp[:, 0:4], in_=rbb[:, 124:128]), reads=RC, writes=[r_bfp])
                S.op('dve', lambda e: e.tensor_scalar(out=bfp[:, 4:8], in0=rbb[:, 124:128], scalar1=pmask[:, 0:1], scalar2=None, op0=ALU.add), reads=RC + [r_bfp], writes=[r_bfp])
                S.op('dve', lambda e: e.tensor_tensor(out=lq[:, 0, :], in0=lq[:, 0, :], in1=lq[:, 1, :], op=ALU.mult), reads=RC, writes=[r_lam])
                S.op('dve', lambda e: e.tensor_tensor(out=lq[:, 2, :], in0=lq[:, 2, :], in1=lq[:, 3, :], op=ALU.mult), reads=[r_lam], writes=[r_lam])
                S.op('act', lambda e: e.activation(out=lq[:, 1, :], in_=lq[:, 0, :], func=AF.Copy, accum_out=lamT[:, 0:1]), reads=[r_lam], writes=[r_lam])
                S.op('act', lambda e: e.activation(out=lq[:, 3, :], in_=lq[:, 2, :], func=AF.Copy, accum_out=lamT[:, 1:2]), reads=[r_lam], writes=[r_lam])
                S.op('act', lambda e: e.activation(out=lamT[:, 2:4], in_=lamT[:, 0:2], func=AF.Exp), reads=[r_lam], writes=[r_lam])
                S.op('dve', lambda e: e.tensor_tensor(out=lamT[:, 4:5], in0=lamT[:, 3:4], in1=lamT[:, 2:3], op=ALU.subtract), reads=[r_lam], writes=[r_lam])
                S.op('dve', lambda e: e.tensor_scalar(out=lamT[:, 5:6], in0=lamT[:, 4:5], scalar1=-LAM_INIT, scalar2=None, op0=ALU.add), reads=[r_lam], writes=[r_lam])
                S.op('dve', lambda e: e.tensor_copy(out=lamT[:, 6:7], in_=lamT[:, 5:6]), reads=[r_lam], writes=[r_lam])

            S.barrier()
            KT = sbt(P2, "KT", [128, 4, 2 * SH], BF16); r_KT = Reg()
            VV = sbt(P2, "VV", [128, NTT, 4, VW], BF16); r_VV = Reg()
            for h in range(4):
                S.dma('sp', lambda e: e.dma_start(out=KT[:, h, :], in_=DKs[h]), reads=[r_dk], pw=[r_KT])
            for t in range(NTT):
                S.dma('sp', lambda e: e.dma_start(out=VV[:, t, :, :], in_=DVs[t * 128:(t + 1) * 128, :].rearrange("p (h e) -> p h e", e=VW)),
                      reads=[r_dv], pw=[r_VV])
            Qp = [[sbt(P2, f"Qp{s_}{m}", [128, 512], BF16) for m in range(2)] for s_ in range(2)]
            r_Qp = [[Reg(), Reg()] for _ in range(2)]
            for s_ in range(2):
                for m in range(2):
                    S.op('dve', lambda e: e.memset(Qp[s_][m][:], 0.0), writes=[r_Qp[s_][m]])
            Pt = [[sbt(P2, f"Pt{m}{b}", [128, 512], BF16) for b in range(2)] for m in range(2)]
            r_Pt = [[Reg(), Reg()] for _ in range(2)]
            tmpS = [sbt(P2, f"tmpS{m}", [128, 128], F32) for m in range(2)]; r_tmpS = [Reg(), Reg()]
            rz = sbt(P2, "rz", [128, 8], F32); r_rz = Reg()
            o1sb = sbt(P2, "o1sb", [128, 128], F32); r_o1 = Reg()
            odsb = sbt(P2, "odsb", [128, 128], F32); r_od = Reg()
            osq2 = sbt(P2, "osqd", [128, 128], F32); r_osq2 = Reg()
            hdt = sbt(P2, "hdt", [128, 4, 128], BF16); r_hdt = Reg()
            pS = [[pst(P2, f"pS{m}{b}", [128, 512], F32) for b in range(2)] for m in range(2)]
            r_pS = [[Reg(True), Reg(True)] for _ in range(2)]
            pO = [pst(P2, f"pO{b}", [128, 512], F32) for b in range(3)]; r_pO = [Reg(True) for _ in range(3)]

            def acc(a, n=130, o=0):
                return pO[a // 3][:, (a % 3) * 160 + o:(a % 3) * 160 + o + n]

            if stop_after == 1.5:
                dbgb = nc.dram_tensor("dbg_biasN", [128, 8, 128], F32, kind="ExternalOutput").ap()
                dbgl = nc.dram_tensor("dbg_lam", [128, 8], F32, kind="ExternalOutput").ap()
                S.dma('sp', lambda e: e.dma_start(out=dbgb, in_=biasN[:]), reads=r_bN, writes=[Reg()])
                S.dma('sp', lambda e: e.dma_start(out=dbgl, in_=lamT[:]), reads=[r_lam], writes=[Reg()])
                S.barrier()
                return nc, S
            it = 0
            for g in range(NT // 4):
                Q0 = NT + 4 * g
                for h in range(4):
                    s_ = it % 2; it += 1
                    S.dma('sp', lambda e: e.dma_start(out=Qp[s_][0][0:64, :], in_=DQs[h, 0:64, g * 512:(g + 1) * 512]), reads=[r_dq], pw=[r_Qp[s_][0]])
                    S.dma('sp', lambda e: e.dma_start(out=Qp[s_][1][64:128, :], in_=DQs[h, 64:128, g * 512:(g + 1) * 512]), reads=[r_dq], pw=[r_Qp[s_][1]])
                    def emit_S(j):
                        r0 = max(0, j - Q0); rf = max(0, j - Q0 + 2)
                        bf = j % 2
                        nears = list(range(r0, min(rf, 4)))
                        for m in range(2):
                            S.op('pe', lambda e: e.matmul(pS[m][bf][:, 128 * r0:512], lhsT=KT[:, h, j * 128:(j + 1) * 128], rhs=Qp[s_][m][:, 128 * r0:512],
                                                          start=True, stop=(len(nears) == 0)), reads=[r_KT, r_Qp[s_][m]], writes=[r_pS[m][bf]])
                            for r in nears:
                                ty = Q0 + r - j
                                S.op('pe', lambda e: e.matmul(pS[m][bf][:, 128 * r:128 * r + 128], lhsT=identb[:], rhs=biasB[:, ty * 4 + h, :],
                                                              start=False, stop=(r == nears[-1])), reads=[r_bB] + RC, writes=[r_pS[m][bf]])

                    def emit_exp(j):
                        r0 = max(0, j - Q0)
                        bf = j % 2
                        bcol = (4 + h) if j < NT else h
                        for m in range(2):
                            S.op('act', lambda e: e.activation(out=Pt[m][bf][:, 128 * r0:512], in_=pS[m][bf][:, 128 * r0:512], func=AF.Exp,
                                                               bias=bfp[:, bcol:bcol + 1], scale=0.125),
                                 reads=[r_pS[m][bf], r_bfp], writes=[r_Pt[m][bf]])

                    def emit_AV(j):
                        r0 = max(0, j - Q0)
                        bf = j % 2
                        for r in range(r0, 4):
                            for m in range(2):
                                a = 2 * r + m
                                last = (j == Q0 + r) and a in (2, 5, 7)
                                S.op('pe', lambda e: e.matmul(acc(a), lhsT=Pt[m][bf][:, 128 * r:128 * r + 128], rhs=VV[:, j, h, 0:130],
                                                              start=(j == 0 and a % 3 == 0), stop=last, skip_group_check=True),
                                     reads=[r_Pt[m][bf], r_VV], writes=[r_pO[a // 3]])

                    NJ = Q0 + 4
                    emit_S(0)
                    for j in range(NJ):
                        if j + 1 < NJ:
                            emit_S(j + 1)
                        emit_exp(j)
                        emit_AV(j)
                    if stop_after == 1.7:
                        dbo = nc.dram_tensor("dbg_pO", [128, 3, 512], F32, kind="ExternalOutput").ap()
                        dbp = nc.dram_tensor("dbg_Pt", [128, 4, 512], BF16, kind="ExternalOutput").ap()
                        dsb = sbt(P2, "dsb", [128, 3, 512], F32); r_dsb = Reg()
                        for b_ in range(3):
                            S.op('act', lambda e: e.activation(out=dsb[:, b_, :], in_=pO[b_][:], func=AF.Copy), reads=[r_pO[b_]], pw=[r_dsb])
                        S.dma('sp', lambda e: e.dma_start(out=dbo, in_=dsb[:]), reads=[r_dsb], writes=[Reg()])
                        for m_ in range(2):
                            for b_ in range(2):
                                S.dma('sp', lambda e: e.dma_start(out=dbp[:, m_ * 2 + b_, :], in_=Pt[m_][b_][:]), reads=[r_Pt[m_][b_]], writes=[Reg()])
                        dbk = nc.dram_tensor("dbg_KT", [128, 4, 2 * SH], BF16, kind="ExternalOutput").ap()
                        dbq = nc.dram_tensor("dbg_Qp", [128, 2, 512], BF16, kind="ExternalOutput").ap()
                        S.dma('sp', lambda e: e.dma_start(out=dbk, in_=KT[:]), reads=[r_KT], writes=[Reg()])
                        for m_ in range(2):
                            S.dma('sp', lambda e: e.dma_start(out=dbq[:, m_, :], in_=Qp[s_][m_][:]), reads=[r_Qp[s_][m_]], writes=[Reg()])
                        S.barrier()
                        return nc, S
                    for r in range(4):
                        a0, a1 = 2 * r, 2 * r + 1
                        S.op('dve', lambda e: e.reciprocal(out=rz[:, 0:1], in_=acc(a0, 1, 128)), reads=[r_pO[a0 // 3]], writes=[r_rz])
                        S.op('dve', lambda e: e.reciprocal(out=rz[:, 1:2], in_=acc(a1, 1, 128)), reads=[r_pO[a1 // 3], r_rz], writes=[r_rz])
                        S.op('dve', lambda e: e.tensor_tensor(out=rz[:, 2:3], in0=rz[:, 1:2], in1=lamT[:, 5:6], op=ALU.mult), reads=[r_rz, r_lam], writes=[r_rz])
                        S.op('act', lambda e: e.activation(out=o1sb[:], in_=acc(a0, 128), func=AF.Copy, scale=rz[:, 0:1]), reads=[r_pO[a0 // 3], r_rz], writes=[r_o1])
                        S.op('dve', lambda e: e.scalar_tensor_tensor(out=odsb[:], in0=acc(a1, 128), scalar=rz[:, 2:3], in1=o1sb[:], op0=ALU.mult, op1=ALU.add),
                             reads=[r_pO[a1 // 3], r_rz, r_o1], writes=[r_od])
                        S.op('act', lambda e: e.activation(out=osq2[:], in_=odsb[:], func=AF.Square, accum_out=rz[:, 3:4]), reads=[r_od, r_rz], writes=[r_osq2, r_rz])
                        S.op('act', lambda e: e.activation(out=rz[:, 4:5], in_=rz[:, 3:4], func=AF.Sqrt, bias=epsT[:, 0:1], scale=1.0 / 128.0), reads=[r_rz] + RC, writes=[r_rz])
                        S.op('dve', lambda e: e.reciprocal(out=rz[:, 5:6], in_=rz[:, 4:5]), reads=[r_rz], writes=[r_rz])
                        S.op('dve', lambda e: e.scalar_tensor_tensor(out=hdt[:, r, :], in0=odsb[:], scalar=rz[:, 5:6], in1=dnwb[:], op0=ALU.mult, op1=ALU.mult),
                             reads=[r_od, r_rz, r_dnwb], pw=[r_hdt])
                    if stop_after == 1.8:
                        S.barrier()
                        return nc, S
                    for r in range(4):
                        S.dma('sp', lambda e: e.dma_start(out=HDs[(4 * g + r) * 128:(4 * g + r + 1) * 128, 512 + h * 128:512 + (h + 1) * 128], in_=hdt[:, r, :]),
                              reads=[r_hdt], pw=[r_hd[4 * g + r]])
                    if stop_after == 1.9:
                        S.barrier()
                        return nc, S
            S.barrier()
        if stop_after <= 2:
            return nc, S

        with ExitStack() as P3:
            wo = sbt(P3, "wo", [128, 8, D], BF16); r_wo = Reg()
            for dc in range(8):
                S.dma('pool', lambda e: e.dma_start(out=wo[:, dc, :], in_=w_out[dc * 128:(dc + 1) * 128, :]), pw=[r_wo])
            rw = sbt(P3, "rw", [128, 8, NEXP], F32); cload('sp', rw[:], router_w.rearrange("(c p) e -> p c e", p=128))
            rbc = sbt(P3, "rbc", [128, NEXP], F32); cload('sp', rbc[:], router_b.partition_broadcast(128))
            ecap = sbt(P3, "ecap", [128, NEXP], F32); cload('sp', ecap[:], c_ecap.partition_broadcast(128))
            sub = sbt(P3, "sub", [128, 128], BF16); cload('pool', sub[:], c_su)
            hb_2 = [sbt(P3, f"hb_{i_}", [128, D], BF16) for i_ in range(2)]; r_hb_2 = [Reg(), Reg()]
            hT_2 = [sbt(P3, f"hT_{i_}", [128, 8, 128], BF16) for i_ in range(2)]; r_hT_2 = [Reg(), Reg()]
            xf_2 = [sbt(P3, f"xf_{i_}", [128, D], F32) for i_ in range(2)]; r_xf_2 = [Reg(), Reg()]
            rr_2 = [sbt(P3, f"rr_{i_}", [128, D], F32) for i_ in range(2)]; r_rr_2 = [Reg(), Reg()]
            x1_2 = [sbt(P3, f"x1_{i_}", [128, D], F32) for i_ in range(2)]; r_x1t_2 = [Reg(), Reg()]
            x1b_2 = [sbt(P3, f"x1b_{i_}", [128, D], BF16) for i_ in range(2)]; r_x1b_2 = [Reg(), Reg()]
            x1T_2 = [sbt(P3, f"x1T_{i_}", [128, 8, 128], F32) for i_ in range(2)]; r_x1T_2 = [Reg(), Reg()]
            rt = sbt(P3, "rt", [128, 8, NEXP], F32); r_rt = Reg()
            t8 = sbt(P3, "t8", [128, 16], F32); r_t8 = Reg()
            mkb = sbt(P3, "mkb", [128, NEXP], BF16); r_mkb = Reg()
            cumb = sbt(P3, "cumb", [128, NEXP], BF16); r_cumb = Reg()
            pT3 = pst(P3, "pT3", [128, 1024], BF16); r_pT3 = Reg(True)
            pA = [pst(P3, f"pA{i}", [128, 512], F32) for i in range(2)]; r_pA = [Reg(True), Reg(True)]
            pB = [pst(P3, f"pB{i}", [128, 512], F32) for i in range(2)]; r_pB = [Reg(True), Reg(True)]
            pL = pst(P3, "pL", [128, 512], F32); r_pL = Reg(True)
            S.op('dve', lambda e: e.memset(rt[:, 6, :], 0.0), writes=[r_rt])
            S.op('dve', lambda e: e.memset(cumb[:], 0.0), writes=[r_cumb])

            def p3_load(i):
                S.dma('sp', lambda e: e.dma_start(out=hb_2[i % 2][:], in_=HDs[i * 128:(i + 1) * 128, :]), reads=[r_hd[i]], writes=[r_hb_2[i % 2]])
                S.dma('sp', lambda e: e.dma_start(out=xf_2[i % 2][:], in_=x_all[SH + i * 128:SH + (i + 1) * 128, :]), writes=[r_xf_2[i % 2]])

            def p3_tile(i):
                hb, r_hb = hb_2[i % 2], r_hb_2[i % 2]; hT, r_hT = hT_2[i % 2], r_hT_2[i % 2]; xf, r_xf = xf_2[i % 2], r_xf_2[i % 2]; rr, r_rr = rr_2[i % 2], r_rr_2[i % 2]; x1, r_x1t = x1_2[i % 2], r_x1t_2[i % 2]; x1b, r_x1b = x1b_2[i % 2], r_x1b_2[i % 2]; x1T, r_x1T = x1T_2[i % 2], r_x1T_2[i % 2];
                if i == 0:
                    p3_load(0)
                if i + 1 < NT:
                    p3_load(i + 1)
                for dc in range(8):
                    S.op('pe', lambda e: e.transpose(out=pT3[:, dc * 128:(dc + 1) * 128], in_=hb[:, dc * 128:(dc + 1) * 128], identity=identb[:]),
                         reads=[r_hb] + RC, writes=[r_pT3])
                S.op('act', lambda e: e.activation(out=hT[:], in_=R3(pT3[:]), func=AF.Copy), reads=[r_pT3], writes=[r_hT])
                for n in range(2):
                    for dc in range(8):
                        S.op('pe', lambda e: e.matmul(pA[n][:], lhsT=hT[:, dc, :], rhs=wo[:, dc, n * 512:(n + 1) * 512], start=(dc == 0), stop=(dc == 7)),
                             reads=[r_hT, r_wo], writes=[r_pA[n]])
                    S.op('dve', lambda e: e.scalar_tensor_tensor(out=rr[:, n * 512:(n + 1) * 512], in0=xf[:, n * 512:(n + 1) * 512], scalar=ALPHA,
                                                                 in1=pA[n][:], op0=ALU.mult, op1=ALU.add), reads=[r_xf, r_pA[n]], pw=[r_rr])
                yield
                layer_norm(rr, r_rr, 0, x1, r_x1t)
                S.dma('sp', lambda e: e.dma_start(out=X1s[i * 128:(i + 1) * 128, :], in_=x1[:]), reads=[r_x1t], writes=[r_x1[i]])
                S.op('act', lambda e: e.activation(out=x1b[:], in_=x1[:], func=AF.Copy), reads=[r_x1t], writes=[r_x1b])
                yield
                for dc in range(8):
                    S.op('pe', lambda e: e.transpose(out=pB[dc // 4][:, (dc % 4) * 128:(dc % 4 + 1) * 128], in_=x1[:, dc * 128:(dc + 1) * 128], identity=ident[:]),
                         reads=[r_x1t] + RC, writes=[r_pB[dc // 4]])
                S.op('act', lambda e: e.activation(out=x1T[:, 0:4, :], in_=R3(pB[0][:]), func=AF.Copy), reads=[r_pB[0]], pw=[r_x1T])
                S.op('dve', lambda e: e.tensor_copy(out=x1T[:, 4:8, :], in_=R3(pB[1][:])), reads=[r_pB[1]], pw=[r_x1T])
                yield
                for dc in range(8):
                    S.op('pe', lambda e: e.matmul(pL[:, 0:NEXP], lhsT=x1T[:, dc, :], rhs=rw[:, dc, :], start=(dc == 0), stop=(dc == 7)),
                         reads=[r_x1T] + RC, writes=[r_pL])
                S.op('dve', lambda e: e.tensor_tensor(out=rt[:, 0, :], in0=pL[:, 0:NEXP], in1=rbc[:], op=ALU.add), reads=[r_pL, r_rt] + RC, writes=[r_rt])
                S.op('dve', lambda e: e.max(out=t8[:, 0:8], in_=rt[:, 0, :]), reads=[r_rt], writes=[r_t8])
                S.op('dve', lambda e: e.tensor_scalar(out=rt[:, 1, :], in0=rt[:, 0, :], scalar1=t8[:, 3:4], scalar2=None, op0=ALU.is_ge), reads=[r_rt, r_t8], writes=[r_rt])
                S.op('dve', lambda e: e.tensor_scalar(out=t8[:, 8:9], in0=t8[:, 0:1], scalar1=-1.0, scalar2=None, op0=ALU.mult), reads=[r_t8], writes=[r_t8])
                S.op('act', lambda e: e.activation(out=rt[:, 2, :], in_=rt[:, 0, :], func=AF.Exp, bias=t8[:, 8:9]), reads=[r_rt, r_t8], writes=[r_rt])
                S.op('dve', lambda e: e.tensor_tensor(out=rt[:, 2, :], in0=rt[:, 2, :], in1=rt[:, 1, :], op=ALU.mult), reads=[r_rt], writes=[r_rt])
                S.op('act', lambda e: e.activation(out=rt[:, 5, :], in_=rt[:, 2, :], func=AF.Copy, accum_out=t8[:, 9:10]), reads=[r_rt, r_t8], writes=[r_rt, r_t8])
                S.op('dve', lambda e: e.reciprocal(out=t8[:, 10:11], in_=t8[:, 9:10]), reads=[r_t8], writes=[r_t8])
                S.op('dve', lambda e: e.tensor_scalar(out=rt[:, 3, :], in0=rt[:, 2, :], scalar1=t8[:, 10:11], scalar2=None, op0=ALU.mult), reads=[r_rt, r_t8], writes=[r_rt])
                S.op('dve', lambda e: e.tensor_copy(out=mkb[:], in_=rt[:, 1, :]), reads=[r_rt], writes=[r_mkb])
                S.op('pe', lambda e: e.matmul(pL[:, 64:64 + NEXP], lhsT=sub[:], rhs=mkb[:], start=True, stop=False), reads=[r_mkb] + RC, writes=[r_pL])
                S.op('pe', lambda e: e.matmul(pL[:, 64:64 + NEXP], lhsT=onesb[:], rhs=cumb[:], start=False, stop=True), reads=[r_cumb] + RC, writes=[r_pL])
                S.op('dve', lambda e: e.tensor_tensor(out=rt[:, 4, :], in0=pL[:, 64:64 + NEXP], in1=ecap[:], op=ALU.add), reads=[r_pL, r_rt] + RC, writes=[r_rt])
                S.op('dve', lambda e: e.tensor_tensor(out=rt[:, 4, :], in0=rt[:, 4, :], in1=rt[:, 1, :], op=ALU.mult), reads=[r_rt], writes=[r_rt])
                S.op('dve', lambda e: e.tensor_tensor(out=rt[:, 6, :], in0=rt[:, 6, :], in1=rt[:, 1, :], op=ALU.add), reads=[r_rt], writes=[r_rt])
                S.op('dve', lambda e: e.tensor_copy(out=cumb[:], in_=rt[:, 6, :]), reads=[r_rt], writes=[r_cumb])
                S.op('dve', lambda e: e.max(out=t8[:, 0:8], in_=rt[:, 4, :]), reads=[r_rt, r_t8], writes=[r_t8])
                S.op('dve', lambda e: e.tensor_scalar(out=posk[:, i, :], in0=t8[:, 0:4], scalar1=-1.0, scalar2=None, op0=ALU.add), reads=[r_t8], writes=[r_posk[i]])
                for k in range(4):
                    S.op('dve', lambda e: e.tensor_scalar(out=rt[:, 5, :], in0=rt[:, 4, :], scalar1=t8[:, k:k + 1], scalar2=None, op0=ALU.is_equal),
                         reads=[r_rt, r_t8], writes=[r_rt])
                    S.op('dve', lambda e: e.tensor_tensor(out=rt[:, 5, :], in0=rt[:, 5, :], in1=rt[:, 3, :], op=ALU.mult), reads=[r_rt], writes=[r_rt])
                    S.op('act', lambda e: e.activation(out=rt[:, 7, :], in_=rt[:, 5, :], func=AF.Copy, accum_out=gk[:, i, k:k + 1]), reads=[r_rt], writes=[r_rt], pw=[r_gk[i]])
                yield
                for k in range(4):
                    S.dma('pool', lambda e: e.indirect_dma_start(out=XSs, out_offset=bass.IndirectOffsetOnAxis(ap=posk[:, i, k:k + 1], axis=0),
                                                                in_=x1b[:], in_offset=None), reads=[r_x1b, r_posk[i]], pw=[r_xs])
            run_staggered(p3_tile, NT, 2)
            S.barrier()
        if stop_after <= 3:
            return nc, S

        NSG = -(-CAP // 512)
        SGW = CAP // NSG
        with ExitStack() as P4:
            wg = [sbt(P4, f"wg{i}", [128, 8, 2 * DEXP], BF16) for i in range(2)]; r_wg = [Reg(), Reg()]
            wd = [sbt(P4, f"wd{i}", [128, 8, D], BF16) for i in range(2)]; r_wd = [Reg(), Reg()]
            bdr = [sbt(P4, f"bdr{i}", [1, D], BF16) for i in range(2)]; r_bdr = [Reg(), Reg()]
            bgu = sbt(P4, "bgu", [128, NEXP * 16], F32); cload('sp', bgu[:], b_gu)
            xs = [sbt(P4, f"xs{i}", [128, D], BF16) for i in range(2)]; r_xsb = [Reg(), Reg()]
            xsT = sbt(P4, "xsT", [128, 8, CAP], BF16); r_xsT = Reg()
            actT = sbt(P4, "actT", [128, 8, CAP], BF16); r_actT = Reg()
            gsb_2 = [sbt(P4, f"gsb{i}", [128, SGW], F32) for i in range(2)]; r_gsb_2 = [Reg(), Reg()]
            ssb_2 = [sbt(P4, f"ssb{i}", [128, SGW], F32) for i in range(2)]; r_ssb_2 = [Reg(), Reg()]
            usb4_2 = [sbt(P4, f"usb4{i}", [128, SGW], F32) for i in range(2)]; r_usb4_2 = [Reg(), Reg()]
            ysb = [sbt(P4, f"ysb{i}", [128, D], BF16) for i in range(2)]; r_ysb = [Reg(), Reg()]
            pT4 = pst(P4, "pT4", [128, 1024], BF16); r_pT4 = Reg(True)
            pGt = [pst(P4, f"pGt{i}", [128, 512], F32) for i in range(2)]; r_pGt = [Reg(True), Reg(True)]
            pUp = [pst(P4, f"pUp{i}", [128, 512], F32) for i in range(2)]; r_pUp = [Reg(True), Reg(True)]
            pY = [pst(P4, f"pY{i}", [128, 512], F32) for i in range(2)]; r_pY = [Reg(True), Reg(True)]

            def load_w(e_):
                b_ = e_ % 2
                for dc in range(8):
                    S.dma('pool', lambda e: e.dma_start(out=wg[b_][:, dc, :], in_=w_gu[e_, dc * 128:(dc + 1) * 128, :]), pw=[r_wg[b_]])
                for dc in range(8):
                    S.dma('pool', lambda e: e.dma_start(out=wd[b_][:, dc, :], in_=w_dn[e_, dc * 128:(dc + 1) * 128, :]), pw=[r_wd[b_]])
                S.dma('pool', lambda e: e.dma_start(out=bdr[b_][:], in_=b_dn[e_:e_ + 1, :]), pw=[r_bdr[b_]])

            load_w(0)
            cnt4 = 0
            ycnt = 0
            for e_ in range(NEXP):
                b_ = e_ % 2
                if e_ + 1 < NEXP:
                    load_w(e_ + 1)
                for s_ in range(NSL):
                    xb_ = xs[s_ % 2]; rxb_ = r_xsb[s_ % 2]
                    S.dma('sp', lambda e: e.dma_start(out=xb_[:], in_=XSs[e_ * CAP + s_ * 128:e_ * CAP + (s_ + 1) * 128, :]), reads=[r_xs], writes=[rxb_])
                    for dc in range(8):
                        S.op('pe', lambda e: e.transpose(out=pT4[:, dc * 128:(dc + 1) * 128], in_=xb_[:, dc * 128:(dc + 1) * 128], identity=identb[:]),
                             reads=[rxb_] + RC, writes=[r_pT4])
                    S.op('act', lambda e: e.activation(out=xsT[:, :, s_ * 128:(s_ + 1) * 128], in_=R3(pT4[:]), func=AF.Copy), reads=[r_pT4], pw=[r_xsT])
                for c in range(8):
                    for sg in range(NSG):
                        sl = slice(sg * SGW, (sg + 1) * SGW)
                        pb_ = cnt4 % 2; cnt4 += 1
                        gsb, r_gsb = gsb_2[pb_], r_gsb_2[pb_]; ssb, r_ssb = ssb_2[pb_], r_ssb_2[pb_]; usb4, r_usb4 = usb4_2[pb_], r_usb4_2[pb_]
                        for dc in range(8):
                            S.op('pe', lambda e: e.matmul(pGt[pb_][:, 0:SGW], lhsT=wg[b_][:, dc, c * 128:(c + 1) * 128], rhs=xsT[:, dc, sl], start=(dc == 0), stop=(dc == 7)),
                                 reads=[r_wg[b_], r_xsT], writes=[r_pGt[pb_]])
                        for dc in range(8):
                            S.op('pe', lambda e: e.matmul(pUp[pb_][:, 0:SGW], lhsT=wg[b_][:, dc, DEXP + c * 128:DEXP + (c + 1) * 128], rhs=xsT[:, dc, sl], start=(dc == 0), stop=(dc == 7)),
                                 reads=[r_wg[b_], r_xsT], writes=[r_pUp[pb_]])
                        S.op('dve', lambda e: e.tensor_scalar(out=gsb[:], in0=pGt[pb_][:, 0:SGW], scalar1=bgu[:, e_ * 16 + c:e_ * 16 + c + 1], scalar2=7.0, op0=ALU.add, op1=ALU.min),
                             reads=[r_pGt[pb_]] + RC, writes=[r_gsb])
                        S.op('act', lambda e: e.activation(out=ssb[:], in_=gsb[:], func=AF.Silu, scale=1.702), reads=[r_gsb], writes=[r_ssb])
                        S.op('dve', lambda e: e.tensor_scalar(out=usb4[:], in0=pUp[pb_][:, 0:SGW], scalar1=bgu[:, e_ * 16 + 8 + c:e_ * 16 + 8 + c + 1], scalar2=7.0, op0=ALU.add, op1=ALU.min),
                             reads=[r_pUp[pb_]] + RC, writes=[r_usb4])
                        S.op('dve', lambda e: e.tensor_scalar(out=usb4[:], in0=usb4[:], scalar1=-7.0, scalar2=1.0, op0=ALU.max, op1=ALU.add), reads=[r_usb4], writes=[r_usb4])
                        S.op('dve', lambda e: e.scalar_tensor_tensor(out=actT[:, c, sl], in0=ssb[:], scalar=1.0 / 1.702, in1=usb4[:], op0=ALU.mult, op1=ALU.mult), reads=[r_ssb, r_usb4], pw=[r_actT])
                for s_ in range(NSL):
                    yb_ = ysb[ycnt % 2]; ryb_ = r_ysb[ycnt % 2]; ycnt += 1
                    for n in range(2):
                        for c in range(8):
                            S.op('pe', lambda e: e.matmul(pY[n][:], lhsT=actT[:, c, s_ * 128:(s_ + 1) * 128], rhs=wd[b_][:, c, n * 512:(n + 1) * 512], start=(c == 0), stop=False),
                                 reads=[r_actT, r_wd[b_]], writes=[r_pY[n]])
                        S.op('pe', lambda e: e.matmul(pY[n][:], lhsT=onesb[0:1, :], rhs=bdr[b_][0:1, n * 512:(n + 1) * 512], start=False, stop=True),
                             reads=[r_bdr[b_]] + RC, writes=[r_pY[n]])
                    S.op('act', lambda e: e.activation(out=yb_[:, 0:512], in_=pY[0][:], func=AF.Copy), reads=[r_pY[0]], pw=[ryb_])
                    S.op('dve', lambda e: e.tensor_copy(out=yb_[:, 512:1024], in_=pY[1][:]), reads=[r_pY[1]], pw=[ryb_])
                    S.dma('sp', lambda e: e.dma_start(out=YSs[e_ * CAP + s_ * 128:e_ * CAP + (s_ + 1) * 128, :], in_=yb_[:]), reads=[ryb_], pw=[r_ys])
            S.barrier()
        if stop_after <= 4:
            return nc, S

        with ExitStack() as P5:
            wgt = sbt(P5, "wgt", [128, 8, D], BF16); r_wgt = Reg()
            for dc in range(8):
                S.dma('pool', lambda e: e.dma_start(out=wgt[:, dc, :], in_=pg_w[dc * 128:(dc + 1) * 128, :]), pw=[r_wgt])
            wpp = sbt(P5, "wpp", [128, 2, D], BF16)
            for dc in range(2):
                S.dma('pool', lambda e: e.dma_start(out=wpp[:, dc, :], in_=pp_w[dc * 128:(dc + 1) * 128, :]), pw=[r_wgt])
            pgbr = sbt(P5, "pgbr", [1, D], BF16)
            S.dma('pool', lambda e: e.dma_start(out=pgbr[:], in_=pg_b), pw=[r_wgt])
            yk_2 = [[sbt(P5, f"yk{k}_{i_}", [128, D], BF16) for k in range(4)] for i_ in range(2)]; r_yk_2 = [[Reg() for _ in range(4)] for _ in range(2)]
            x1t_2 = [sbt(P5, f"x1t_{i_}", [128, D], F32) for i_ in range(2)]; r_x1r_2 = [Reg(), Reg()]
            rr5_2 = [sbt(P5, f"rr5_{i_}", [128, D], F32) for i_ in range(2)]; r_rr5_2 = [Reg(), Reg()]
            x2_2 = [sbt(P5, f"x2_{i_}", [128, D], F32) for i_ in range(2)]; r_x2_2 = [Reg(), Reg()]
            x2b_2 = [sbt(P5, f"x2b_{i_}", [128, D], BF16) for i_ in range(2)]; r_x2b_2 = [Reg(), Reg()]
            x2T_2 = [sbt(P5, f"x2T_{i_}", [128, 8, 128], BF16) for i_ in range(2)]; r_x2T_2 = [Reg(), Reg()]
            sgs_2 = [sbt(P5, f"sgs_{i_}", [128, D], F32) for i_ in range(2)]; r_sgs_2 = [Reg(), Reg()]
            pbt_2 = [sbt(P5, f"pbt_{i_}", [128, PLE], BF16) for i_ in range(2)]; r_pbt_2 = [Reg(), Reg()]
            pTt_2 = [sbt(P5, f"pTt_{i_}", [128, 2, 128], BF16) for i_ in range(2)]; r_pTt_2 = [Reg(), Reg()]
            r3_2 = [sbt(P5, f"r3_{i_}", [128, D], F32) for i_ in range(2)]; r_r3_2 = [Reg(), Reg()]
            ot_2 = [sbt(P5, f"ot_{i_}", [128, D], F32) for i_ in range(2)]; r_ot_2 = [Reg(), Reg()]
            pT5 = pst(P5, "pT5", [128, 1024], BF16); r_pT5 = Reg(True)
            pA5 = [pst(P5, f"pA5{i}", [128, 512], F32) for i in range(2)]; r_pA5 = [Reg(True), Reg(True)]
            pB5 = [pst(P5, f"pB5{i}", [128, 512], F32) for i in range(2)]; r_pB5 = [Reg(True), Reg(True)]
            def p5_load(i):
                for k in range(4):
                    S.dma('pool', lambda e: e.indirect_dma_start(out=yk_2[i % 2][k][:], out_offset=None, in_=YSs,
                                                                in_offset=bass.IndirectOffsetOnAxis(ap=posk[:, i, k:k + 1], axis=0)),
                          reads=[r_ys, r_posk[i]], writes=[r_yk_2[i % 2][k]])
                S.dma('sp', lambda e: e.dma_start(out=x1t_2[i % 2][:], in_=X1s[i * 128:(i + 1) * 128, :]), reads=[r_x1[i]], writes=[r_x1r_2[i % 2]])
                S.dma('pool', lambda e: e.dma_start(out=pbt_2[i % 2][:], in_=p_own[i * 128:(i + 1) * 128, :]), writes=[r_pbt_2[i % 2]])

            def p5_tile(i):
                yk, r_yk = yk_2[i % 2], r_yk_2[i % 2]
                x1t, r_x1r = x1t_2[i % 2], r_x1r_2[i % 2]; rr5, r_rr5 = rr5_2[i % 2], r_rr5_2[i % 2]; x2, r_x2 = x2_2[i % 2], r_x2_2[i % 2]; x2b, r_x2b = x2b_2[i % 2], r_x2b_2[i % 2]; x2T, r_x2T = x2T_2[i % 2], r_x2T_2[i % 2]; sgs, r_sgs = sgs_2[i % 2], r_sgs_2[i % 2]; pbt, r_pbt = pbt_2[i % 2], r_pbt_2[i % 2]; pTt, r_pTt = pTt_2[i % 2], r_pTt_2[i % 2]; r3, r_r3 = r3_2[i % 2], r_r3_2[i % 2]; ot, r_ot = ot_2[i % 2], r_ot_2[i % 2];
                if i == 0:
                    p5_load(0)
                if i + 1 < NT:
                    p5_load(i + 1)
                S.op('dve', lambda e: e.tensor_scalar(out=rr5[:], in0=x1t[:], scalar1=ALPHA, scalar2=None, op0=ALU.mult), reads=[r_x1r], writes=[r_rr5])
                for k in range(4):
                    S.op('dve', lambda e: e.scalar_tensor_tensor(out=rr5[:], in0=yk[k][:], scalar=gk[:, i, k:k + 1], in1=rr5[:], op0=ALU.mult, op1=ALU.add),
                         reads=[r_yk[k], r_gk[i], r_rr5], writes=[r_rr5])
                yield
                layer_norm(rr5, r_rr5, 1, x2, r_x2)
                S.op('act', lambda e: e.activation(out=x2b[:], in_=x2[:], func=AF.Copy), reads=[r_x2], writes=[r_x2b])
                yield
                for dc in range(8):
                    S.op('pe', lambda e: e.transpose(out=pT5[:, dc * 128:(dc + 1) * 128], in_=x2b[:, dc * 128:(dc + 1) * 128], identity=identb[:]),
                         reads=[r_x2b] + RC, writes=[r_pT5])
                S.op('act', lambda e: e.activation(out=x2T[:], in_=R3(pT5[:]), func=AF.Copy), reads=[r_pT5], writes=[r_x2T])
                for dc in range(2):
                    S.op('pe', lambda e: e.transpose(out=pT5[:, dc * 128:(dc + 1) * 128], in_=pbt[:, dc * 128:(dc + 1) * 128], identity=identb[:]),
                         reads=[r_pbt] + RC, writes=[r_pT5])
                S.op('act', lambda e: e.activation(out=pTt[:], in_=R3(pT5[:, 0:256]), func=AF.Copy), reads=[r_pT5], writes=[r_pTt])
                yield
                for n in range(2):
                    for dc in range(8):
                        S.op('pe', lambda e: e.matmul(pA5[n][:], lhsT=x2T[:, dc, :], rhs=wgt[:, dc, n * 512:(n + 1) * 512], start=(dc == 0), stop=False),
                             reads=[r_x2T, r_wgt], writes=[r_pA5[n]])
                    S.op('pe', lambda e: e.matmul(pA5[n][:], lhsT=onesb[0:1, :], rhs=pgbr[0:1, n * 512:(n + 1) * 512], start=False, stop=True),
                         reads=[r_wgt] + RC, writes=[r_pA5[n]])
                    S.op('act', lambda e: e.activation(out=sgs[:, n * 512:(n + 1) * 512], in_=pA5[n][:], func=AF.Sigmoid), reads=[r_pA5[n]], pw=[r_sgs])
                    for dc in range(2):
                        S.op('pe', lambda e: e.matmul(pB5[n][:], lhsT=pTt[:, dc, :], rhs=wpp[:, dc, n * 512:(n + 1) * 512], start=(dc == 0), stop=(dc == 1)),
                             reads=[r_pTt, r_wgt], writes=[r_pB5[n]])
                    S.op('dve', lambda e: e.tensor_tensor(out=r3[:, n * 512:(n + 1) * 512], in0=pB5[n][:], in1=sgs[:, n * 512:(n + 1) * 512], op=ALU.mult),
                         reads=[r_pB5[n], r_sgs], pw=[r_r3])
                yield
                S.op('dve', lambda e: e.scalar_tensor_tensor(out=r3[:], in0=x2[:], scalar=ALPHA, in1=r3[:], op0=ALU.mult, op1=ALU.add),
                     reads=[r_x2, r_r3], writes=[r_r3])
                layer_norm(r3, r_r3, 2, ot, r_ot)
                S.dma('sp', lambda e: e.dma_start(out=y_out[i * 128:(i + 1) * 128, :], in_=ot[:]), reads=[r_ot], pw=[r_yout])
            run_staggered(p5_tile, NT, 2)
            S.barrier()
    return nc, S


def make_inputs(inputs, SH, CAP):
    f = lambda a: np.ascontiguousarray(np.asarray(a, dtype=np.float32))
    x = f(inputs['x']); p = f(inputs['p'])
    B = x.shape[0]
    cst = host_consts()
    shared = {
        'w_in': f(inputs['w_in'][0]), 'conv_w': np.ascontiguousarray(f(inputs['conv_w'][0]).reshape(4, 12, 128).transpose(2, 1, 0)),
        'gdn_a_log': f(inputs['gdn_a_log']), 'gdn_dt_bias': f(inputs['gdn_dt_bias']),
        'gdn_norm_w': f(inputs['gdn_norm_w']), 'diff_norm_w': f(inputs['diff_norm_w']),
        'diff_lq1': f(inputs['diff_lq1']), 'diff_lk1': f(inputs['diff_lk1']),
        'diff_lq2': f(inputs['diff_lq2']), 'diff_lk2': f(inputs['diff_lk2']),
        'w_out': f(inputs['w_out'][0]), 'rel_bias': f(inputs['rel_bias']).reshape(1, 128),
        'ln1_g': f(inputs['ln1_g']), 'ln1_b': f(inputs['ln1_b']), 'ln2_g': f(inputs['ln2_g']), 'ln2_b': f(inputs['ln2_b']),
        'ln3_g': f(inputs['ln3_g']), 'ln3_b': f(inputs['ln3_b']),
        'router_w': f(inputs['router_w'][0]), 'router_b': f(inputs['router_b']),
        'w_gate_up': f(inputs['w_gate_up'][0]), 'b_gate_up': np.ascontiguousarray(f(inputs['b_gate_up'][0]).reshape(NEXP * 16, 128).T),
        'w_down': f(inputs['w_down'][0]), 'b_down': f(inputs['b_down'][0]),
        'ple_gate_w': f(inputs['ple_gate_w'][0]), 'ple_gate_b': f(inputs['ple_gate_b']), 'ple_proj_w': f(inputs['ple_proj_w'][0]),
        'c_ident': cst['ident'], 'c_trichi': cst['trichi'], 'c_blk': cst['blk'], 'c_negt': cst['negt'], 'c_smask': cst['smask'],
        'c_su': cst['su'], 'c_t5oh': cst['t5oh'], 'c_negd': cst['negd'],
        'c_ecap': (np.arange(NEXP, dtype=np.float32) * CAP + 1.0).reshape(1, NEXP),
    }
    maps = []
    for c in range(2 * B):
        b, hf = c // 2, c % 2
        if hf == 0:
            xa = np.concatenate([np.zeros((SH, D), np.float32), x[b, :SH]], axis=0)
            pm = np.full((128, 1), NEG, np.float32)
        else:
            xa = x[b]
            pm = np.zeros((128, 1), np.float32)
        m = dict(shared)
        m['x_all'] = np.ascontiguousarray(xa)
        m['p_own'] = np.ascontiguousarray(p[0, b, hf * SH:(hf + 1) * SH])
        m['pmask'] = pm
        maps.append(m)
    return maps


_NC_CACHE = {}


def kernel(**inputs):
    x = np.asarray(inputs['x'])
    B, SEQ, _ = x.shape
    SH = SEQ // 2
    CAP = 768 if SH >= 4096 else 256
    key = (SH, CAP)
    if key not in _NC_CACHE:
        _NC_CACHE[key] = build(SH, CAP)[0]
    nc = _NC_CACHE[key]
    maps = make_inputs(inputs, SH, CAP)
    res = run_bass_kernel_spmd(nc, maps, core_ids=list(range(2 * B)))
    out = np.zeros((B, SEQ, D), np.float32)
    for c in range(2 * B):
        out[c // 2, (c % 2) * SH:(c % 2 + 1) * SH] = res.results[c]['y_out']
    return out
```

```python
import math
from contextlib import ExitStack
import numpy as np
import concourse.bass as bass
import concourse.mybir as mybir
from concourse.alu_op_type import AluOpType as ALU
from concourse.bass_utils import run_bass_kernel_spmd

AF = mybir.ActivationFunctionType
F32 = mybir.dt.float32
BF16 = mybir.dt.bfloat16
I32 = mybir.dt.int32

ENGS = ('pe', 'act', 'pool', 'dve', 'sp')
SEM_LIMIT = 30000
RING = 8
RINGS = {'sp': 16, 'pool': 24, 'act': 8}


class Reg:
    __slots__ = ('w', 'pc', 'pd', 'rc', 'rd', 'excl')

    def __init__(self, excl=False):
        self.excl = excl
        self.w = None
        self.pc = {}
        self.pd = set()
        self.rc = {}
        self.rd = set()


class Sched:
    def __init__(self, nc, same_engine_sync=True):
        self.nc = nc
        self.e = {'pe': nc.tensor, 'act': nc.scalar, 'pool': nc.gpsimd, 'dve': nc.vector, 'sp': nc.sync}
        self.cnt = {k: 0 for k in ENGS}
        self.sems = {k: [] for k in ENGS}
        self.clock = {k: {f: 0 for f in ENGS} for k in ENGS}
        self.snap = {k: [] for k in ENGS}
        self.dknown = {k: set() for k in ENGS}
        self.dq = {q: {'n': 0, 'sems': []} for q in ('sp', 'pool', 'act')}
        self.same = same_engine_sync
        self.nwait = 0
        self.ninst = 0

    def _csem(self, eng, n):
        ep = (n - 1) // SEM_LIMIT
        while len(self.sems[eng]) <= ep:
            self.sems[eng].append(self.nc.alloc_semaphore(name=f"c_{eng}_{len(self.sems[eng])}"))
        return self.sems[eng][ep], (n - 1) % SEM_LIMIT + 1

    def _dsem(self, q, i):
        d = self.dq[q]
        R = RINGS[q]
        while len(d['sems']) < R:
            d['sems'].append(self.nc.alloc_semaphore(name=f"d_{q}_{len(d['sems'])}"))
        return d['sems'][i % R], 16 * (i // R + 1)

    def _wait(self, E, tok):
        if tok[0] == 'c':
            _, F, n = tok
            if F == E and (E == 'pe' or not self.same):
                return
            if self.clock[E][F] >= n:
                return
            sem, val = self._csem(F, n)
            self.e[E].wait_ge(sem, val)
            self.nwait += 1
            self.clock[E][F] = n
            sn = self.snap[F][n - 1]
            ce = self.clock[E]
            for g, v in sn.items():
                if v > ce[g]:
                    ce[g] = v
        else:
            _, q, i = tok
            if (q, i) in self.dknown[E]:
                return
            sem, val = self._dsem(q, i)
            self.e[E].wait_ge(sem, val)
            self.nwait += 1
            self.dknown[E].add((q, i))

    def _deps(self, E, reads, writes, pw):
        toks = []
        for r in reads:
            if r.w is not None:
                toks.append(r.w)
            for f, n in r.pc.items():
                toks.append(('c', f, n))
            toks.extend(r.pd)
        for w in writes:
            if w.w is not None:
                toks.append(w.w)
            for f, n in w.pc.items():
                toks.append(('c', f, n))
            toks.extend(w.pd)
            for f, n in w.rc.items():
                toks.append(('c', f, n))
            toks.extend(w.rd)
        for w in pw:
            if w.w is not None:
                toks.append(w.w)
            for f, n in w.rc.items():
                toks.append(('c', f, n))
            toks.extend(w.rd)
        for t in toks:
            self._wait(E, t)

    def _post(self, tok, reads, writes, pw):
        for r in reads:
            if tok[0] == 'c':
                if r.rc.get(tok[1], 0) < tok[2]:
                    r.rc[tok[1]] = tok[2]
            else:
                r.rd.add(tok)
        for w in writes:
            w.w = tok
            w.pc = {}
            w.pd = set()
            w.rc = {}
            w.rd = set()
        for w in pw:
            if tok[0] == 'c':
                if w.pc.get(tok[1], 0) < tok[2]:
                    w.pc[tok[1]] = tok[2]
            else:
                w.pd.add(tok)

    def op(self, E, fn, reads=(), writes=(), pw=()):
        if any(r.excl for r in reads) or any(r.excl for r in pw):
            writes = list(writes) + [r for r in reads if r.excl] + [r for r in pw if r.excl]
            reads = [r for r in reads if not r.excl]
            pw = [r for r in pw if not r.excl]
        self._deps(E, reads, writes, pw)
        inst = fn(self.e[E])
        n = self.cnt[E] + 1
        sem, _ = self._csem(E, n)
        inst.then_inc(sem, 1)
        self.cnt[E] = n
        self.snap[E].append(dict(self.clock[E]))
        self.ninst += 1
        tok = ('c', E, n)
        self._post(tok, reads, writes, pw)
        return tok

    def dma(self, q, fn, reads=(), writes=(), pw=()):
        E = q
        self._deps(E, reads, writes, pw)
        d = self.dq[q]
        i = d['n']
        if i >= RINGS[q]:
            self._wait(E, ('d', q, i - RINGS[q]))
        sem, _ = self._dsem(q, i)
        inst = fn(self.e[E])
        inst.then_inc(sem, 16)
        d['n'] = i + 1
        self.ninst += 1
        tok = ('d', q, i)
        self._post(tok, reads, writes, pw)
        return tok

    def barrier(self):
        for E in ENGS:
            for F in ENGS:
                if F != E and self.cnt[F] > 0:
                    self._wait(E, ('c', F, self.cnt[F]))
                elif F == E and E != 'pe' and self.cnt[F] > 0:
                    self._wait(E, ('c', F, self.cnt[F]))
            self.drain(E)

    def drain(self, E):
        for q in self.dq:
            d = self.dq[q]
            for i in range(max(0, d['n'] - RINGS[q]), d['n']):
                self._wait(E, ('d', q, i))


D = 1024
GH = 4
DH = 4
PLE = 256
NEXP = 32
DEXP = 1024
DINP = 3592
C_GQ, C_GK, C_GV, C_A, C_B, C_Z, C_DQ, C_DK, C_DV = 0, 512, 1024, 1536, 1540, 1544, 2056, 2568, 3080
ALPHA = 2 ** 0.25
LAM_INIT = 0.8 - 0.6 * math.exp(0.0)
NEG = -30000.0


def t5_bucket_np(d):
    d = np.maximum(d, 0)
    large = 16 + (np.log(np.maximum(d, 1).astype(np.float32) / np.float32(16)) / np.float32(math.log(8.0))
                  * np.float32(16)).astype(np.int32)
    large = np.minimum(large, 31)
    return np.where(d < 16, d, large)


def host_consts():
    i = np.arange(128)
    ch = i // 64
    c = {}
    c['ident'] = np.eye(128, dtype=np.float32)
    same = ch[:, None] == ch[None, :]
    tri = (same & (i[:, None] <= i[None, :])).astype(np.float32)
    chi = (ch[:, None] == np.arange(2)[None, :]).astype(np.float32)
    c['trichi'] = np.concatenate([tri, chi], axis=1)
    c['blk'] = same.astype(np.float32)
    c['negt'] = np.where(same & (i[None, :] >= i[:, None]), 0.0, NEG).astype(np.float32)
    c['smask'] = (1.0 - np.eye(128)).astype(np.float32)
    c['su'] = (i[:, None] < i[None, :]).astype(np.float32)
    oh = np.zeros((128, 64, 128), np.float32)
    for ty in range(2):
        dmat = i[None, :] - i[:, None] + 128 * ty
        bk = t5_bucket_np(dmat)
        for b in range(32):
            oh[:, ty * 32 + b, :] = ((bk == b) & (dmat >= 0)).astype(np.float32)
    c['t5oh'] = oh
    c['negd'] = np.where(i[None, :] >= i[:, None], 0.0, NEG).astype(np.float32)
    return c


def build(SH, CAP, stop_after=99, dbg=False):
    F32R = mybir.dt.float32r
    NT = SH // 128
    NTT = 2 * NT
    NSL = CAP // 128
    VW = 136
    assert NT % 4 == 0
    nc = bass.Bass("TRN2", target_bir_lowering=False)
    S = Sched(nc)

    def din(name, shape, dt=F32):
        return nc.dram_tensor(name, list(shape), dt, kind="ExternalInput").ap()

    def dscr(name, shape, dt):
        return nc.dram_tensor(name, list(shape), dt, kind=("ExternalOutput" if dbg else "Internal")).ap()

    x_all = din("x_all", [2 * SH, D])
    p_own = din("p_own", [SH, PLE])
    pmask_d = din("pmask", [128, 1])
    w_in = din("w_in", [D, DINP])
    conv_w = din("conv_w", [128, 12, 4])
    a_log = din("gdn_a_log", [1, 4]); dt_bias = din("gdn_dt_bias", [1, 4])
    gnw = din("gdn_norm_w", [1, 128]); dnw = din("diff_norm_w", [1, 128])
    lqk = [din(n, [1, 64]) for n in ("diff_lq1", "diff_lk1", "diff_lq2", "diff_lk2")]
    w_out = din("w_out", [D, D])
    rel_bias = din("rel_bias", [1, 128])
    ln_g = [din(f"ln{k}_g", [1, D]) for k in (1, 2, 3)]
    ln_b = [din(f"ln{k}_b", [1, D]) for k in (1, 2, 3)]
    router_w = din("router_w", [D, NEXP]); router_b = din("router_b", [1, NEXP])
    w_gu = din("w_gate_up", [NEXP, D, 2 * DEXP]); b_gu = din("b_gate_up", [128, NEXP * 16])
    w_dn = din("w_down", [NEXP, DEXP, D]); b_dn = din("b_down", [NEXP, D])
    pg_w = din("ple_gate_w", [D, D]); pg_b = din("ple_gate_b", [1, D]); pp_w = din("ple_proj_w", [PLE, D])
    c_ident = din("c_ident", [128, 128]); c_trichi = din("c_trichi", [128, 130]); c_blk = din("c_blk", [128, 128])
    c_negt = din("c_negt", [128, 128]); c_smask = din("c_smask", [128, 128]); c_su = din("c_su", [128, 128])
    c_t5oh = din("c_t5oh", [128, 64, 128]); c_negd = din("c_negd", [128, 128]); c_ecap = din("c_ecap", [1, NEXP])
    y_out = nc.dram_tensor("y_out", [SH, D], F32, kind="ExternalOutput").ap()

    DQs = dscr("s_dq", [4, 128, SH], BF16)
    DKs = dscr("s_dk", [4, 128, 2 * SH], BF16)
    DVs = dscr("s_dv", [2 * SH, 4 * VW], BF16)
    HDs = dscr("s_heads", [SH, D], BF16)
    X1s = dscr("s_x1", [SH, D], F32)
    XSs = dscr("s_xslots", [NEXP * CAP, D], BF16)
    YSs = dscr("s_yslots", [NEXP * CAP, D], BF16)
    r_dq = Reg(); r_dk = Reg(); r_dv = Reg()
    r_hd = [Reg() for _ in range(NT)]
    r_x1 = [Reg() for _ in range(NT)]
    r_xs = Reg()
    r_ys = Reg()
    r_yout = Reg()

    def R3(ap, b=128):
        return ap.rearrange("p (a b) -> p a b", b=b)

    with ExitStack() as G:
        def sbt(st, name, shape, dt):
            return st.enter_context(nc.sbuf_tensor(name, list(shape), dt))

        def pst(st, name, shape, dt):
            return st.enter_context(nc.psum_tensor(name, list(shape), dt))

        r_c = Reg()
        RC = [r_c]

        def cload(q, out_ap, in_ap, **kw):
            S.dma(q, lambda e: e.dma_start(out=out_ap, in_=in_ap, **kw), pw=[r_c])

        ident = sbt(G, "ident", [128, 128], F32)
        identb = sbt(G, "identb", [128, 128], BF16)
        onesb = sbt(G, "onesb", [128, 128], BF16)
        onesf = sbt(G, "onesf", [128, 128], F32)
        lnG = [sbt(G, f"lnG{k}", [128, D], F32) for k in range(3)]
        lnB = [sbt(G, f"lnB{k}", [128, D], F32) for k in range(3)]
        posk = sbt(G, "posk", [128, NT, 4], I32); r_posk = [Reg() for _ in range(NT)]
        gk = sbt(G, "gk", [128, NT, 4], F32); r_gk = [Reg() for _ in range(NT)]
        pmask = sbt(G, "pmaskt", [128, 1], F32)
        stat = sbt(G, "stat", [128, 16], F32); r_stat = Reg()
        epsT = sbt(G, "epsT", [128, 4], F32)
        S.op('dve', lambda e: e.memset(epsT[:, 0:1], 1e-6), pw=[r_c])
        S.op('dve', lambda e: e.memset(epsT[:, 1:2], 1e-5), pw=[r_c])
        S.op('dve', lambda e: e.memset(epsT[:, 2:3], 1.0), pw=[r_c])
        S.op('dve', lambda e: e.memset(epsT[:, 3:4], 0.0), pw=[r_c])
        cload('sp', ident[:], c_ident)
        cload('pool', identb[:], c_ident)
        cload('sp', pmask[:], pmask_d)
        S.op('dve', lambda e: e.memset(onesf[:], 1.0), pw=[r_c])
        S.op('dve', lambda e: e.memset(onesb[:], 1.0), pw=[r_c])
        for k in range(3):
            cload('sp', lnG[k][:], ln_g[k].partition_broadcast(128))
            cload('sp', lnB[k][:], ln_b[k].partition_broadcast(128))


        def run_staggered(gen_fn, n, lag):
            active = []; steps = {}; nxt = 0
            while nxt < n or active:
                if nxt < n and len(active) < 2 and (not active or steps[id(active[-1])] >= lag):
                    g_ = gen_fn(nxt); active.append(g_); steps[id(g_)] = 0; nxt += 1
                for g_ in list(active):
                    try:
                        next(g_); steps[id(g_)] += 1
                    except StopIteration:
                        active.remove(g_)

        def layer_norm(src, src_reg, k, dst, dst_reg):
            for hh in range(2):
                S.op('dve', lambda e: e.bn_stats(out=stat[:, 6 * hh:6 * hh + 6], in_=src[:, 512 * hh:512 * hh + 512]),
                     reads=[src_reg], writes=[r_stat] if hh == 0 else [], pw=[] if hh == 0 else [r_stat])
            S.op('dve', lambda e: e.bn_aggr(out=stat[:, 12:14], in_=stat[:, 0:12]), reads=[r_stat], pw=[r_stat])
            S.op('act', lambda e: e.activation(out=stat[:, 15:16], in_=stat[:, 13:14], func=AF.Sqrt, bias=epsT[:, 1:2]), reads=[r_stat] + RC, pw=[r_stat])
            S.op('dve', lambda e: e.reciprocal(out=stat[:, 14:15], in_=stat[:, 15:16]), reads=[r_stat], pw=[r_stat])
            S.op('dve', lambda e: e.tensor_scalar(out=src[:], in0=src[:], scalar1=stat[:, 12:13], scalar2=stat[:, 14:15],
                                                  op0=ALU.subtract, op1=ALU.mult), reads=[src_reg, r_stat], writes=[src_reg])
            S.op('dve', lambda e: e.tensor_tensor(out=src[:], in0=src[:], in1=lnG[k][:], op=ALU.mult),
                 reads=[src_reg] + RC, writes=[src_reg])
            S.op('dve', lambda e: e.tensor_tensor(out=dst[:], in0=src[:], in1=lnB[k][:], op=ALU.add),
                 reads=[src_reg] + RC, writes=[dst_reg])

        with ExitStack() as P1:
            wb = sbt(P1, "wb", [128, 8, DINP], BF16); r_wb = Reg()
            for dc in range(8):
                S.dma('pool', lambda e: e.dma_start(out=wb[:, dc, :], in_=w_in[dc * 128:(dc + 1) * 128, :]), pw=[r_wb])
            trichi = sbt(P1, "trichi", [128, 130], F32); cload('sp', trichi[:], c_trichi)
            blk = sbt(P1, "blk", [128, 128], F32); cload('sp', blk[:], c_blk)
            negt = sbt(P1, "negt", [128, 128], F32); cload('sp', negt[:], c_negt)
            smask = sbt(P1, "smask", [128, 128], F32); cload('sp', smask[:], c_smask)
            cw = sbt(P1, "cw", [128, 12, 4], F32)
            cload('sp', cw[:], conv_w)
            gpar = sbt(P1, "gpar", [128, 12], F32); r_gpar = Reg()
            cload('sp', gpar[:, 0:4], a_log.partition_broadcast(128))
            cload('sp', gpar[:, 4:8], dt_bias.partition_broadcast(128))
            S.op('act', lambda e: e.activation(out=gpar[:, 8:12], in_=gpar[:, 0:4], func=AF.Exp), reads=RC, writes=[r_gpar])
            S.op('dve', lambda e: e.tensor_scalar(out=gpar[:, 8:12], in0=gpar[:, 8:12], scalar1=-1.0, scalar2=None, op0=ALU.mult),
                 reads=[r_gpar], writes=[r_gpar])
            gnwb = sbt(P1, "gnwb", [128, 128], F32); cload('sp', gnwb[:], gnw.partition_broadcast(128))

            xb = [sbt(P1, f"xb{i}", [128, D], BF16) for i in range(2)]; r_xb = [Reg(), Reg()]
            xT = sbt(P1, "xT", [128, 8, 128], BF16); r_xT = Reg()
            cb = sbt(P1, "cb", [128, 12, 131], F32); r_cb = Reg()
            cacc = sbt(P1, "cacc", [128, 12, 128], F32); r_cacc = Reg()
            qkv = sbt(P1, "qkv", [128, 12, 128], F32); r_qkv = Reg()
            sq = sbt(P1, "sq", [128, 8, 128], BF16); r_sq = Reg()
            rn = sbt(P1, "rn", [128, 8, 128], F32); r_rn = Reg()
            qkn_2 = [sbt(P1, f"qkn{i}", [128, 8, 128], BF16) for i in range(2)]; r_qkn_2 = [Reg(), Reg()]
            vsb = sbt(P1, "vsb", [128, 4, 128], BF16); r_vsb = Reg()
            ktok_2 = [sbt(P1, f"ktok{i}", [128, 4, 2, 128], BF16) for i in range(2)]; r_ktok_2 = [Reg(), Reg()]
            vtok_2 = [sbt(P1, f"vtok{i}", [128, 4, 128], BF16) for i in range(2)]; r_vtok_2 = [Reg(), Reg()]
            gt_2 = [sbt(P1, f"gt{i}", [128, 32], F32) for i in range(2)]; r_gt_2 = [Reg(), Reg()]
            dqk = sbt(P1, "dqk", [128, 8, 128], BF16); r_dqk = Reg()
            dvz = sbt(P1, "dvz", [128, 4, VW], BF16); r_dvz = Reg()
            zs_2 = [sbt(P1, f"zs{i}", [128, 512], F32) for i in range(2)]; r_zs_2 = [Reg(), Reg()]
            gbc = [sbt(P1, f"gbc{h}", [128, 2, 128], F32) for h in range(4)]; r_gbc = [Reg() for _ in range(4)]
            dect = [sbt(P1, f"dect{h}", [128, 128], F32) for h in range(4)]; r_dect = [Reg() for _ in range(4)]
            decs = [sbt(P1, f"decs{h}", [128, 128], F32) for h in range(4)]; r_decs = [Reg() for _ in range(4)]
            brow = [sbt(P1, f"brow{h}", [128, 128], F32) for h in range(4)]; r_brow = [Reg() for _ in range(4)]
            egrow = [sbt(P1, f"egrow{h}", [128, 128], F32) for h in range(4)]; r_egrow = [Reg() for _ in range(4)]
            glt = [sbt(P1, f"glt{h}", [128, 2], F32) for h in range(4)]; r_glt = [Reg() for _ in range(4)]
            Xm = [[sbt(P1, f"Xm{h}{i}", [128, 128], F32) for i in range(2)] for h in range(4)]; r_Xm = [[Reg(), Reg()] for _ in range(4)]
            Ym = [[sbt(P1, f"Ym{h}{i}", [128, 128], F32) for i in range(2)] for h in range(4)]; r_Ym = [[Reg(), Reg()] for _ in range(4)]
            Tt = [[sbt(P1, f"Tt{h}{i}", [128, 128], F32) for i in range(2)] for h in range(4)]; r_Tt = [[Reg(), Reg()] for _ in range(4)]
            Ttb = [sbt(P1, f"Ttb{h}", [128, 128], BF16) for h in range(4)]; r_Ttb = [Reg() for _ in range(4)]
            intT = [sbt(P1, f"intT{h}", [128, 128], BF16) for h in range(4)]; r_intT = [Reg() for _ in range(4)]
            usb = [sbt(P1, f"usb{h}", [128, 128], F32) for h in range(4)]; r_usb = [Reg() for _ in range(4)]
            wT = [sbt(P1, f"wT{h}", [128, 128], BF16) for h in range(4)]; r_wT = [Reg() for _ in range(4)]
            qd = [sbt(P1, f"qd{h}", [128, 2, 128], BF16) for h in range(4)]; r_qd = [Reg() for _ in range(4)]
            vnp = [sbt(P1, f"vnp{h}", [128, 2, 128], BF16) for h in range(4)]; r_vnp = [Reg() for _ in range(4)]
            Sf = [sbt(P1, f"Sf{h}", [128, 128], F32) for h in range(4)]; r_Sf = [Reg() for _ in range(4)]
            Sb = [sbt(P1, f"Sb{h}", [128, 2, 128], BF16) for h in range(4)]; r_Sb = [[Reg(), Reg()] for _ in range(4)]
            osb = [sbt(P1, f"osb{h}", [128, 128], F32) for h in range(4)]; r_osb = [Reg() for _ in range(4)]
            osq = [sbt(P1, f"osq{h}", [128, 128], F32) for h in range(4)]; r_osq = [Reg() for _ in range(4)]
            ost = [sbt(P1, f"ost{h}", [128, 4], F32) for h in range(4)]; r_ost = [Reg() for _ in range(4)]
            hg_2 = [sbt(P1, f"hg{i}", [128, 512], BF16) for i in range(2)]; r_hg_2 = [Reg(), Reg()]

            pT = pst(P1, "pT", [128, 1024], BF16); r_pT = Reg(True)
            pT2 = pst(P1, "pT2", [128, 1024], BF16); r_pT2 = Reg(True)
            pF1 = pst(P1, "pF", [128, 512], F32); pF = [pF1, pF1]; _rpf = Reg(True); r_pF = [_rpf, _rpf]
            pM = pst(P1, "pM", [128, 512], F32); r_pM = Reg(True)
            pH = [pst(P1, f"pH{i}", [128, 512], F32) for i in range(4)]
            r_pH = [Reg(True) for _ in range(4)]
            SLOTA = [0, 128, 258]
            SLOTB = [0, 128, 256, 384]

            def pg(b, s, n=128):
                o = SLOTB[s] if b == 2 else SLOTA[s]
                return pG[b][:, o:o + n]

            S.op('dve', lambda e: e.memset(dvz[:].rearrange("p a b -> p (a b)"), 1.0), writes=[r_dvz])
            S.op('dve', lambda e: e.memset(cb[:].rearrange("p a b -> p (a b)"), 0.0), writes=[r_cb])
            for h in range(4):
                S.op('dve', lambda e: e.memset(qd[h][:].rearrange("p a b -> p (a b)"), 0.0), writes=[r_qd[h]])
                S.op('dve', lambda e: e.memset(vnp[h][:].rearrange("p a b -> p (a b)"), 0.0), writes=[r_vnp[h]])
            for h in range(4):
                S.op('dve', lambda e: e.memset(Sf[h][:], 0.0), writes=[r_Sf[h]])
                S.op('dve', lambda e: e.memset(Sb[h][:].rearrange("p a b -> p (a b)"), 0.0), writes=r_Sb[h])

            def load_x(t):
                S.dma('pool', lambda e: e.dma_start(out=xb[t % 2][:], in_=x_all[t * 128:(t + 1) * 128, :]), writes=[r_xb[t % 2]])

            load_x(0)
            fm_groups = [(C_GQ, 0), (C_GK, 1), (C_GV, 2), (C_DQ, 3), (C_DK, 4)]
            def front_gen(t):
                gt, r_gt = gt_2[t % 2], r_gt_2[t % 2]; qkn, r_qkn = qkn_2[t % 2], r_qkn_2[t % 2]; ktok, r_ktok = ktok_2[t % 2], r_ktok_2[t % 2]
                vtok, r_vtok = vtok_2[t % 2], r_vtok_2[t % 2]; zs, r_zs = zs_2[t % 2], r_zs_2[t % 2]
                own = t >= NT
                ti = t - NT
                if t + 1 < NTT:
                    load_x(t + 1)
                for dc in range(8):
                    S.op('pe', lambda e: e.transpose(out=pT[:, dc * 128:(dc + 1) * 128], in_=xb[t % 2][:, dc * 128:(dc + 1) * 128], identity=identb[:]),
                         reads=[r_xb[t % 2]] + RC, writes=[r_pT])
                S.op('act', lambda e: e.activation(out=xT[:, 0:4, :], in_=R3(pT[:, 0:512]), func=AF.Copy), reads=[r_pT], pw=[r_xT])
                S.op('dve', lambda e: e.tensor_copy(out=xT[:, 4:8, :], in_=R3(pT[:, 512:1024])), reads=[r_pT], pw=[r_xT])
                yield
                gi = 0
                for (c0, gidx) in fm_groups:
                    if gidx == 0 and not (own or t == NT - 1):
                        continue
                    if gidx == 3 and not own:
                        continue
                    pf = pF[gi % 2]; rpf = r_pF[gi % 2]; gi += 1
                    for h in range(4):
                        for dc in range(8):
                            S.op('pe', lambda e: e.matmul(pf[:, h * 128:(h + 1) * 128], lhsT=wb[:, dc, c0 + h * 128:c0 + (h + 1) * 128],
                                                          rhs=xT[:, dc, :], start=(dc == 0), stop=(dc == 7)),
                                 reads=[r_wb, r_xT], writes=[rpf])
                    if gidx < 3:
                        S.op('act', lambda e: e.activation(out=cb[:, gidx * 4:gidx * 4 + 4, 3:131], in_=R3(pf[:]), func=AF.Copy),
                             reads=[rpf], pw=[r_cb])
                        yield
                    else:
                        o0 = 0 if gidx == 3 else 4
                        S.op('act', lambda e: e.activation(out=dqk[:, o0:o0 + 4, :], in_=R3(pf[:]), func=AF.Copy), reads=[rpf], pw=[r_dqk])
                        yield
                for dc in range(8):
                    S.op('pe', lambda e: e.matmul(pM[:, :], lhsT=xT[:, dc, :], rhs=wb[:, dc, C_DV:C_DV + 512], start=(dc == 0), stop=(dc == 7)),
                         reads=[r_wb, r_xT], writes=[r_pM])
                S.op('dve', lambda e: e.tensor_copy(out=dvz[:, :, 0:128], in_=R3(pM[:])), reads=[r_pM], pw=[r_dvz])
                yield
                S.dma('sp', lambda e: e.dma_start(out=DVs[t * 128:(t + 1) * 128, :], in_=dvz[:].rearrange("p a b -> p (a b)")), reads=[r_dvz], pw=[r_dv])
                for h in range(4):
                    S.dma('sp', lambda e: e.dma_start(out=DKs[h, :, t * 128:(t + 1) * 128], in_=dqk[:, 4 + h, :]), reads=[r_dqk], pw=[r_dk])
                    if own:
                        S.dma('sp', lambda e: e.dma_start(out=DQs[h, :, ti * 128:(ti + 1) * 128], in_=dqk[:, h, :]), reads=[r_dqk], pw=[r_dq])
                if own:
                    for dc in range(8):
                        S.op('pe', lambda e: e.matmul(pM[:, :], lhsT=xT[:, dc, :], rhs=wb[:, dc, C_Z:C_Z + 512], start=(dc == 0), stop=(dc == 7)),
                             reads=[r_wb, r_xT], writes=[r_pM])
                    S.op('act', lambda e: e.activation(out=zs[:], in_=pM[:], func=AF.Silu), reads=[r_pM], writes=[r_zs])
                    yield
                for dc in range(8):
                    S.op('pe', lambda e: e.matmul(pM[:, 0:8], lhsT=xT[:, dc, :], rhs=wb[:, dc, C_A:C_A + 8], start=(dc == 0), stop=(dc == 7)),
                         reads=[r_wb, r_xT], writes=[r_pM])
                S.op('dve', lambda e: e.tensor_tensor(out=gt[:, 0:4], in0=pM[:, 0:4], in1=gpar[:, 4:8], op=ALU.add),
                     reads=[r_pM, r_gpar] + RC, writes=[r_gt])
                yield
                S.op('act', lambda e: e.activation(out=gt[:, 0:4], in_=gt[:, 0:4], func=AF.Exp), reads=[r_gt], writes=[r_gt])
                S.op('act', lambda e: e.activation(out=gt[:, 0:4], in_=gt[:, 0:4], func=AF.Ln, bias=epsT[:, 2:3]), reads=[r_gt] + RC, writes=[r_gt])
                S.op('dve', lambda e: e.tensor_tensor(out=gt[:, 0:4], in0=gt[:, 0:4], in1=gpar[:, 8:12], op=ALU.mult),
                     reads=[r_gt, r_gpar], writes=[r_gt])
                S.op('act', lambda e: e.activation(out=gt[:, 4:8], in_=pM[:, 4:8], func=AF.Sigmoid), reads=[r_pM, r_gt], writes=[r_gt])
                S.op('dve', lambda e: e.tensor_scalar(out=gt[:, 28:32], in0=gt[:, 4:8], scalar1=-1.0, scalar2=None, op0=ALU.mult),
                     reads=[r_gt], writes=[r_gt])
                yield
                S.op('pe', lambda e: e.matmul(pM[:, 16:20], lhsT=trichi[:, 0:128], rhs=gt[:, 0:4], start=True, stop=True),
                     reads=[r_gt] + RC, writes=[r_pM])
                S.op('pe', lambda e: e.matmul(pM[:, 20:24], lhsT=blk[:], rhs=gt[:, 0:4], start=True, stop=True),
                     reads=[r_gt] + RC, pw=[r_pM])
                S.op('dve', lambda e: e.tensor_copy(out=gt[:, 8:16], in_=pM[:, 16:24]), reads=[r_pM, r_gt], writes=[r_gt])
                yield
                S.op('act', lambda e: e.activation(out=gt[:, 16:20], in_=gt[:, 8:12], func=AF.Exp), reads=[r_gt], writes=[r_gt])
                S.op('dve', lambda e: e.tensor_tensor(out=gt[:, 20:24], in0=gt[:, 12:16], in1=gt[:, 8:12], op=ALU.subtract), reads=[r_gt], writes=[r_gt])
                S.op('act', lambda e: e.activation(out=gt[:, 20:24], in_=gt[:, 20:24], func=AF.Exp), reads=[r_gt], writes=[r_gt])
                S.op('dve', lambda e: e.tensor_scalar(out=gt[:, 24:28], in0=gt[:, 8:12], scalar1=-1.0, scalar2=None, op0=ALU.mult), reads=[r_gt], writes=[r_gt])
                yield
                c_lo = 0 if own else 4
                ncn = 12 - c_lo
                for k in range(4):
                    in1 = cw[:, c_lo:12, k:k + 1].to_broadcast([128, ncn, 128])
                    if k == 0:
                        S.op('dve', lambda e: e.tensor_tensor(out=cacc[:, c_lo:12, :], in0=cb[:, c_lo:12, 0:128], in1=in1, op=ALU.mult),
                             reads=[r_cb] + RC, writes=[r_cacc])
                    else:
                        S.op('pool', lambda e: e.tensor_tensor(out=qkv[:, c_lo:12, :], in0=cb[:, c_lo:12, k:k + 128], in1=in1, op=ALU.mult),
                             reads=[r_cb] + RC, writes=[r_qkv])
                        S.op('dve', lambda e: e.tensor_tensor(out=cacc[:, c_lo:12, :], in0=cacc[:, c_lo:12, :], in1=qkv[:, c_lo:12, :], op=ALU.add),
                             reads=[r_cacc, r_qkv], writes=[r_cacc])
                    yield
                S.op('pool', lambda e: e.tensor_copy(out=cb[:, :, 0:3], in_=cb[:, :, 128:131]), reads=[r_cb], pw=[r_cb])
                S.op('act', lambda e: e.activation(out=qkv[:, c_lo:12, :], in_=cacc[:, c_lo:12, :], func=AF.Silu), reads=[r_cacc], writes=[r_qkv])
                yield
                S.op('dve', lambda e: e.tensor_tensor(out=sq[:, c_lo:8, :], in0=qkv[:, c_lo:8, :], in1=qkv[:, c_lo:8, :], op=ALU.mult),
                     reads=[r_qkv], writes=[r_sq])
                yield
                for half in ([0, 1] if own else [1]):
                    pf = pF[half]; rpf = r_pF[half]
                    S.op('pe', lambda e: e.matmul(pf[:, :], lhsT=onesb[:], rhs=sq[:, half * 4:half * 4 + 4, :], start=True, stop=True),
                         reads=[r_sq] + RC, writes=[rpf])
                    S.op('act', lambda e: e.activation(out=rn[:, half * 4:half * 4 + 4, :], in_=R3(pf[:]), func=AF.Sqrt, bias=epsT[:, 0:1]),
                         reads=[rpf] + RC, pw=[r_rn])
                    S.op('dve', lambda e: e.reciprocal(out=rn[:, half * 4:half * 4 + 4, :], in_=rn[:, half * 4:half * 4 + 4, :]),
                         reads=[r_rn], pw=[r_rn])
                    yield
                if own:
                    S.op('dve', lambda e: e.scalar_tensor_tensor(out=qkn[:, 0:4, :], in0=qkv[:, 0:4, :], scalar=128.0 ** -0.5, in1=rn[:, 0:4, :],
                                                                 op0=ALU.mult, op1=ALU.mult), reads=[r_qkv, r_rn], pw=[r_qkn])
                S.op('pool', lambda e: e.tensor_tensor(out=qkn[:, 4:8, :], in0=qkv[:, 4:8, :], in1=rn[:, 4:8, :], op=ALU.mult),
                     reads=[r_qkv, r_rn], pw=[r_qkn])
                S.op('pool', lambda e: e.tensor_copy(out=vsb[:], in_=qkv[:, 8:12, :]), reads=[r_qkv], writes=[r_vsb])
                yield
                for h in range(4):
                    S.op('pe', lambda e: e.transpose(out=pT2[:, h * 128:(h + 1) * 128], in_=qkn[:, 4 + h, :], identity=identb[:]),
                         reads=[r_qkn] + RC, writes=[r_pT2])
                    S.op('pe', lambda e: e.transpose(out=pT2[:, 512 + h * 128:512 + (h + 1) * 128], in_=vsb[:, h, :], identity=identb[:]),
                         reads=[r_vsb] + RC, writes=[r_pT2])
                for h in range(4):
                    S.op('dve', lambda e: e.tensor_scalar(out=ktok[:, h, 0, :], in0=pT2[:, h * 128:(h + 1) * 128], scalar1=gt[:, 20 + h:21 + h], scalar2=None, op0=ALU.mult),
                         reads=[r_pT2, r_gt], pw=[r_ktok])
                    S.op('dve', lambda e: e.tensor_scalar(out=ktok[:, h, 1, :], in0=pT2[:, h * 128:(h + 1) * 128], scalar1=gt[:, 16 + h:17 + h], scalar2=None, op0=ALU.mult),
                         reads=[r_pT2, r_gt], pw=[r_ktok])
                S.op('act', lambda e: e.activation(out=vtok[:], in_=R3(pT2[:, 512:1024]), func=AF.Copy), reads=[r_pT2], writes=[r_vtok])
                yield

            def head_gen(t, h):
                own = t >= NT
                gt, r_gt = gt_2[t % 2], r_gt_2[t % 2]; qkn, r_qkn = qkn_2[t % 2], r_qkn_2[t % 2]; ktok, r_ktok = ktok_2[t % 2], r_ktok_2[t % 2]
                vtok, r_vtok = vtok_2[t % 2], r_vtok_2[t % 2]; zs, r_zs = zs_2[t % 2], r_zs_2[t % 2]; hg, r_hg = hg_2[t % 2], r_hg_2[t % 2]
                bank = pH[h]; rb = r_pH[h]
                A = bank[:, 0:128]; B = bank[:, 128:258]; Bm = bank[:, 128:256]; Bgl = bank[:, 256:258]; C = bank[:, 258:386]
                S.op('dve', lambda e: e.tensor_scalar(out=gbc[h][:, 0, :], in0=onesf[:], scalar1=gt[:, h:h + 1], scalar2=None, op0=ALU.mult),
                     reads=[r_gt] + RC, pw=[r_gbc[h]])
                S.op('dve', lambda e: e.tensor_scalar(out=gbc[h][:, 1, :], in0=onesf[:], scalar1=gt[:, 4 + h:5 + h], scalar2=None, op0=ALU.mult),
                     reads=[r_gt] + RC, pw=[r_gbc[h]])
                yield
                S.op('pe', lambda e: e.matmul(A, lhsT=gbc[h][:, 0, :], rhs=trichi[:, 0:128], start=True, stop=False), reads=[r_gbc[h]] + RC, writes=[rb])
                S.op('pe', lambda e: e.matmul(A, lhsT=ident[:], rhs=negt[:], start=False, stop=True), reads=RC, writes=[rb])
                S.op('pe', lambda e: e.matmul(B, lhsT=gbc[h][:, 0, :], rhs=trichi[:, 0:130], start=True, stop=True), reads=[r_gbc[h]] + RC, writes=[rb])
                S.op('pe', lambda e: e.matmul(C, lhsT=gbc[h][:, 1, :], rhs=ident[:], start=True, stop=True), reads=[r_gbc[h]] + RC, writes=[rb])
                yield
                S.op('act', lambda e: e.activation(out=dect[h][:], in_=A, func=AF.Exp, bias=gt[:, 24 + h:25 + h]), reads=[rb, r_gt], writes=[r_dect[h]])
                S.op('act', lambda e: e.activation(out=glt[h][:], in_=Bgl, func=AF.Exp), reads=[rb], writes=[r_glt[h]])
                if own:
                    S.op('act', lambda e: e.activation(out=egrow[h][:], in_=Bm, func=AF.Exp), reads=[rb], writes=[r_egrow[h]])
                S.op('act', lambda e: e.activation(out=brow[h][:], in_=C, func=AF.Copy), reads=[rb], writes=[r_brow[h]])
                yield
                S.op('pool', lambda e: e.tensor_tensor(out=decs[h][:], in0=dect[h][:], in1=smask[:], op=ALU.mult), reads=[r_dect[h]] + RC, writes=[r_decs[h]])
                S.op('pe', lambda e: e.matmul(A, lhsT=qkn[:, 4 + h, :], rhs=qkn[:, 4 + h, :], start=True, stop=True), reads=[r_qkn], writes=[rb])
                if own:
                    S.op('pe', lambda e: e.matmul(Bm, lhsT=qkn[:, 4 + h, :], rhs=qkn[:, h, :], start=True, stop=True), reads=[r_qkn], writes=[rb])
                yield
                S.op('dve', lambda e: e.scalar_tensor_tensor(out=Ym[h][0][:], in0=A, scalar=gt[:, 28 + h:29 + h], in1=decs[h][:],
                                                             op0=ALU.mult, op1=ALU.mult), reads=[rb, r_gt, r_decs[h]], writes=[r_Ym[h][0]])
                if own:
                    S.op('dve', lambda e: e.tensor_tensor(out=intT[h][:], in0=Bm, in1=dect[h][:], op=ALU.mult), reads=[rb, r_dect[h]], writes=[r_intT[h]])
                    for c in range(2):
                        S.op('pool', lambda e: e.tensor_tensor(out=qd[h][:, c, c * 64:(c + 1) * 64], in0=qkn[:, h, c * 64:(c + 1) * 64],
                                                               in1=egrow[h][:, c * 64:(c + 1) * 64], op=ALU.mult), reads=[r_qkn, r_egrow[h]], pw=[r_qd[h]])
                yield
                S.op('pe', lambda e: e.transpose(out=C, in_=Ym[h][0][:], identity=ident[:]), reads=[r_Ym[h][0]] + RC, writes=[rb])
                S.op('pool', lambda e: e.tensor_tensor(out=Tt[h][0][:], in0=Ym[h][0][:], in1=ident[:], op=ALU.add), reads=[r_Ym[h][0]] + RC, writes=[r_Tt[h][0]])
                yield
                S.op('act', lambda e: e.activation(out=Xm[h][0][:], in_=C, func=AF.Copy), reads=[rb], writes=[r_Xm[h][0]])
                yield
                S.op('pe', lambda e: e.matmul(A, lhsT=Ym[h][0][:], rhs=Xm[h][0][:], start=True, stop=True), reads=[r_Ym[h][0], r_Xm[h][0]], writes=[rb])
                S.op('pe', lambda e: e.matmul(Bm, lhsT=Xm[h][0][:], rhs=Ym[h][0][:], start=True, stop=True), reads=[r_Ym[h][0], r_Xm[h][0]], writes=[rb])
                yield
                for k in range(5):
                    a, b = k % 2, (k + 1) % 2
                    if k > 0:
                        S.op('dve', lambda e: e.tensor_tensor(out=Tt[h][a][:], in0=C, in1=Tt[h][b][:], op=ALU.add), reads=[rb, r_Tt[h][b]], writes=[r_Tt[h][a]])
                    S.op('act', lambda e: e.activation(out=Xm[h][b][:], in_=A, func=AF.Copy), reads=[rb], writes=[r_Xm[h][b]])
                    if k < 4:
                        S.op('dve', lambda e: e.tensor_copy(out=Ym[h][b][:], in_=Bm), reads=[rb], writes=[r_Ym[h][b]])
                    yield
                    S.op('pe', lambda e: e.matmul(C, lhsT=Xm[h][b][:], rhs=Tt[h][a][:], start=True, stop=True), reads=[r_Xm[h][b], r_Tt[h][a]], writes=[rb])
                    if k < 4:
                        S.op('pe', lambda e: e.matmul(A, lhsT=Ym[h][b][:], rhs=Xm[h][b][:], start=True, stop=True), reads=[r_Ym[h][b], r_Xm[h][b]], writes=[rb])
                    if k < 3:
                        S.op('pe', lambda e: e.matmul(Bm, lhsT=Xm[h][b][:], rhs=Ym[h][b][:], start=True, stop=True), reads=[r_Ym[h][b], r_Xm[h][b]], writes=[rb])
                    yield
                S.op('dve', lambda e: e.tensor_tensor(out=Tt[h][1][:], in0=C, in1=Tt[h][0][:], op=ALU.add), reads=[rb, r_Tt[h][0]], writes=[r_Tt[h][1]])
                S.op('dve', lambda e: e.tensor_tensor(out=Ttb[h][:], in0=brow[h][:], in1=Tt[h][1][:], op=ALU.mult), reads=[r_brow[h], r_Tt[h][1]], writes=[r_Ttb[h]])
                yield
                S.op('pe', lambda e: e.matmul(A, lhsT=Ttb[h][:], rhs=vtok[:, h, :], start=True, stop=True), reads=[r_Ttb[h], r_vtok], writes=[rb])
                S.op('pe', lambda e: e.matmul(Bm, lhsT=ktok[:, h, 1, :], rhs=Ttb[h][:], start=True, stop=True), reads=[r_Ttb[h], r_ktok], writes=[rb])
                yield
                S.op('act', lambda e: e.activation(out=usb[h][:], in_=A, func=AF.Copy), reads=[rb], writes=[r_usb[h]])
                S.op('dve', lambda e: e.tensor_copy(out=wT[h][:], in_=Bm), reads=[rb], writes=[r_wT[h]])
                yield
                for c in range(2):
                    lo, hi = c * 64, (c + 1) * 64
                    S.op('pe', lambda e: e.matmul(A, lhsT=wT[h][:], rhs=Sb[h][:, c, :], start=True, stop=True), reads=[r_wT[h], r_Sb[h][c]], writes=[rb])
                    yield
                    S.op('dve', lambda e: e.tensor_tensor(out=vnp[h][lo:hi, c, :], in0=usb[h][lo:hi, :], in1=bank[lo:hi, 0:128], op=ALU.subtract),
                         reads=[r_usb[h], rb], pw=[r_vnp[h]])
                    yield
                    S.op('pe', lambda e: e.matmul(Bm, lhsT=ktok[:, h, 0, :], rhs=vnp[h][:, c, :], start=True, stop=True), reads=[r_ktok, r_vnp[h]], writes=[rb])
                    if own and c == 1:
                        S.op('pe', lambda e: e.matmul(C, lhsT=qd[h][:, 0, :], rhs=Sb[h][:, 0, :], start=True, stop=False), reads=[r_qd[h], r_Sb[h][0]], writes=[rb])
                        S.op('pe', lambda e: e.matmul(C, lhsT=qd[h][:, 1, :], rhs=Sb[h][:, 1, :], start=False, stop=False), reads=[r_qd[h], r_Sb[h][1]], writes=[rb])
                        S.op('pe', lambda e: e.matmul(C, lhsT=intT[h][:], rhs=vnp[h][:, 0, :], start=False, stop=False), reads=[r_intT[h], r_vnp[h]], writes=[rb])
                        S.op('pe', lambda e: e.matmul(C, lhsT=intT[h][:], rhs=vnp[h][:, 1, :], start=False, stop=True), reads=[r_intT[h], r_vnp[h]], writes=[rb])
                    yield
                    S.op('dve', lambda e: e.scalar_tensor_tensor(out=Sf[h][:], in0=Sf[h][:], scalar=glt[h][:, c:c + 1], in1=Bm,
                                                                 op0=ALU.mult, op1=ALU.add), reads=[r_Sf[h], r_glt[h], rb], writes=[r_Sf[h]])
                    S.op('act', lambda e: e.activation(out=Sb[h][:, 1 - c, :], in_=Sf[h][:], func=AF.Copy), reads=[r_Sf[h]], writes=[r_Sb[h][1 - c]])
                    yield
                if own:
                    S.op('act', lambda e: e.activation(out=osb[h][:], in_=C, func=AF.Copy), reads=[rb], writes=[r_osb[h]])
                    S.op('act', lambda e: e.activation(out=osq[h][:], in_=osb[h][:], func=AF.Square, accum_out=ost[h][:, 0:1]),
                         reads=[r_osb[h]], writes=[r_osq[h], r_ost[h]])
                    S.op('act', lambda e: e.activation(out=ost[h][:, 1:2], in_=ost[h][:, 0:1], func=AF.Sqrt, bias=epsT[:, 0:1], scale=1.0 / 128.0),
                         reads=[r_ost[h]] + RC, writes=[r_ost[h]])
                    yield
                    S.op('dve', lambda e: e.reciprocal(out=ost[h][:, 2:3], in_=ost[h][:, 1:2]), reads=[r_ost[h]], writes=[r_ost[h]])
                    S.op('dve', lambda e: e.scalar_tensor_tensor(out=osq[h][:], in0=osb[h][:], scalar=ost[h][:, 2:3], in1=gnwb[:], op0=ALU.mult, op1=ALU.mult),
                         reads=[r_osb[h], r_ost[h]] + RC, writes=[r_osq[h]])
                    yield
                    S.op('pool', lambda e: e.tensor_tensor(out=hg[:, h * 128:(h + 1) * 128], in0=osq[h][:], in1=zs[:, h * 128:(h + 1) * 128], op=ALU.mult),
                         reads=[r_osq[h], r_zs], pw=[r_hg])


            for t in range(NTT + 1):
                gens = []
                if t < NTT:
                    gens.append(front_gen(t))
                if t >= 1:
                    gens += [head_gen(t - 1, h) for h in range(4)]
                while gens:
                    for g_ in list(gens):
                        try:
                            next(g_)
                        except StopIteration:
                            gens.remove(g_)
                if t >= 1 and (t - 1) >= NT:
                    ti = t - 1 - NT
                    S.dma('sp', lambda e: e.dma_start(out=HDs[ti * 128:(ti + 1) * 128, 0:512], in_=hg_2[(t - 1) % 2][:]), reads=[r_hg_2[(t - 1) % 2]], pw=[r_hd[ti]])
            S.barrier()
        if stop_after <= 1:
            return nc, S

        lamT = sbt(G, "lamT", [128, 8], F32); r_lam = Reg()
        with ExitStack() as P2:
            biasN = sbt(P2, "biasN", [128, 8, 128], F32); r_bN = [Reg() for _ in range(8)]
            biasB = sbt(P2, "biasB", [128, 8, 128], BF16); r_bB = Reg()
            rbb = sbt(P2, "rbb", [128, 128], F32); cload('sp', rbb[:], rel_bias.partition_broadcast(128))
            bfp = sbt(P2, "bfp", [128, 8], F32); r_bfp = Reg()
            dnwb = sbt(P2, "dnwb", [128, 128], F32); cload('sp', dnwb[:], dnw.partition_broadcast(128)); r_dnwb = Reg()
            S.op('dve', lambda e: e.tensor_scalar(out=dnwb[:], in0=dnwb[:], scalar1=1.0 - LAM_INIT, scalar2=None, op0=ALU.mult), reads=RC, writes=[r_dnwb])
            with ExitStack() as P2a:
                t5 = sbt(P2a, "t5", [128, 64, 128], F32); cload('sp', t5[:], c_t5oh)
                negd = sbt(P2a, "negd", [128, 128], F32); cload('sp', negd[:], c_negd)
                lq = sbt(P2a, "lq", [128, 4, 64], F32)
                for k in range(4):
                    cload('sp', lq[:, k, :], lqk[k].partition_broadcast(128))
                for ty in range(2):
                    for h in range(4):
                        dst = biasN[:, ty * 4 + h, :]
                        rb_ = r_bN[ty * 4 + h]
                        S.op('dve', lambda e: e.tensor_scalar(out=dst, in0=t5[:, ty * 32, :], scalar1=rbb[:, h:h + 1], scalar2=None, op0=ALU.mult),
                             reads=RC, writes=[rb_])
                        for b in range(1, 32):
                            S.op('dve', lambda e: e.scalar_tensor_tensor(out=dst, in0=t5[:, ty * 32 + b, :], scalar=rbb[:, b * 4 + h:b * 4 + h + 1], in1=dst,
                                                                         op0=ALU.mult, op1=ALU.add), reads=RC + [rb_], writes=[rb_])
                        if ty == 0:
                            S.op('dve', lambda e: e.tensor_tensor(out=dst, in0=dst, in1=negd[:], op=ALU.add), reads=RC + [rb_], writes=[rb_])
                for ty in range(2):
                    for h in range(4):
                        S.op('dve', lambda e: e.tensor_scalar(out=biasB[:, ty * 4 + h, :], in0=biasN[:, ty * 4 + h, :], scalar1=rbb[:, 124 + h:125 + h], scalar2=8.0,
                                                              op0=ALU.subtract, op1=ALU.mult), reads=RC + [r_bN[ty * 4 + h]], pw=[r_bB])
                S.op('dve', lambda e: e.tensor_copy(out=bfp[:, 0:4], in_=rbb[:, 124:128]), reads=RC, writes=[r_bfp])
                S.op('dve', lambda e: e.tensor_scalar(out=bfp[:, 4:8], in0=rbb[:, 124:128], scalar1=pmask[:, 0:1], scalar2=None, op0=ALU.add), reads=RC + [r_bfp], writes=[r_bfp])
                S.op('dve', lambda e: e.tensor_tensor(out=lq[:, 0, :], in0=lq[:, 0, :], in1=lq[:, 1, :], op=ALU.mult), reads=RC, writes=[r_lam])
                S.op('dve', lambda e: e.tensor_tensor(out=lq[:, 2, :], in0=lq[:, 2, :], in1=lq[:, 3, :], op=ALU.mult), reads=[r_lam], writes=[r_lam])
                S.op('act', lambda e: e.activation(out=lq[:, 1, :], in_=lq[:, 0, :], func=AF.Copy, accum_out=lamT[:, 0:1]), reads=[r_lam], writes=[r_lam])
                S.op('act', lambda e: e.activation(out=lq[:, 3, :], in_=lq[:, 2, :], func=AF.Copy, accum_out=lamT[:, 1:2]), reads=[r_lam], writes=[r_lam])
                S.op('act', lambda e: e.activation(out=lamT[:, 2:4], in_=lamT[:, 0:2], func=AF.Exp), reads=[r_lam], writes=[r_lam])
                S.op('dve', lambda e: e.tensor_tensor(out=lamT[:, 4:5], in0=lamT[:, 3:4], in1=lamT[:, 2:3], op=ALU.subtract), reads=[r_lam], writes=[r_lam])
                S.op('dve', lambda e: e.tensor_scalar(out=lamT[:, 5:6], in0=lamT[:, 4:5], scalar1=-LAM_INIT, scalar2=None, op0=ALU.add), reads=[r_lam], writes=[r_lam])
                S.op('dve', lambda e: e.tensor_copy(out=lamT[:, 6:7], in_=lamT[:, 5:6]), reads=[r_lam], writes=[r_lam])

            S.barrier()
            KT = sbt(P2, "KT", [128, 4, 2 * SH], BF16); r_KT = Reg()
            VV = sbt(P2, "VV", [128, NTT, 4, VW], BF16); r_VV = Reg()
            for h in range(4):
                S.dma('sp', lambda e: e.dma_start(out=KT[:, h, :], in_=DKs[h]), reads=[r_dk], pw=[r_KT])
            for t in range(NTT):
                S.dma('sp', lambda e: e.dma_start(out=VV[:, t, :, :], in_=DVs[t * 128:(t + 1) * 128, :].rearrange("p (h e) -> p h e", e=VW)),
                      reads=[r_dv], pw=[r_VV])
            Qp = [[sbt(P2, f"Qp{s_}{m}", [128, 512], BF16) for m in range(2)] for s_ in range(2)]
            r_Qp = [[Reg(), Reg()] for _ in range(2)]
            for s_ in range(2):
                for m in range(2):
                    S.op('dve', lambda e: e.memset(Qp[s_][m][:], 0.0), writes=[r_Qp[s_][m]])
            Pt = [[sbt(P2, f"Pt{m}{b}", [128, 512], BF16) for b in range(2)] for m in range(2)]
            r_Pt = [[Reg(), Reg()] for _ in range(2)]
            tmpS = [sbt(P2, f"tmpS{m}", [128, 128], F32) for m in range(2)]; r_tmpS = [Reg(), Reg()]
            rz = sbt(P2, "rz", [128, 8], F32); r_rz = Reg()
            o1sb = sbt(P2, "o1sb", [128, 128], F32); r_o1 = Reg()
            odsb = sbt(P2, "odsb", [128, 128], F32); r_od = Reg()
            osq2 = sbt(P2, "osqd", [128, 128], F32); r_osq2 = Reg()
            hdt = sbt(P2, "hdt", [128, 4, 128], BF16); r_hdt = Reg()
            pS = [[pst(P2, f"pS{m}{b}", [128, 512], F32) for b in range(2)] for m in range(2)]
            r_pS = [[Reg(True), Reg(True)] for _ in range(2)]
            pO = [pst(P2, f"pO{b}", [128, 512], F32) for b in range(3)]; r_pO = [Reg(True) for _ in range(3)]

            def acc(a, n=130, o=0):
                return pO[a // 3][:, (a % 3) * 160 + o:(a % 3) * 160 + o + n]

            if stop_after == 1.5:
                dbgb = nc.dram_tensor("dbg_biasN", [128, 8, 128], F32, kind="ExternalOutput").ap()
                dbgl = nc.dram_tensor("dbg_lam", [128, 8], F32, kind="ExternalOutput").ap()
                S.dma('sp', lambda e: e.dma_start(out=dbgb, in_=biasN[:]), reads=r_bN, writes=[Reg()])
                S.dma('sp', lambda e: e.dma_start(out=dbgl, in_=lamT[:]), reads=[r_lam], writes=[Reg()])
                S.barrier()
                return nc, S
            it = 0
            for g in range(NT // 4):
                Q0 = NT + 4 * g
                for h in range(4):
                    s_ = it % 2; it += 1
                    S.dma('sp', lambda e: e.dma_start(out=Qp[s_][0][0:64, :], in_=DQs[h, 0:64, g * 512:(g + 1) * 512]), reads=[r_dq], pw=[r_Qp[s_][0]])
                    S.dma('sp', lambda e: e.dma_start(out=Qp[s_][1][64:128, :], in_=DQs[h, 64:128, g * 512:(g + 1) * 512]), reads=[r_dq], pw=[r_Qp[s_][1]])
                    def emit_S(j):
                        r0 = max(0, j - Q0); rf = max(0, j - Q0 + 2)
                        bf = j % 2
                        nears = list(range(r0, min(rf, 4)))
                        for m in range(2):
                            S.op('pe', lambda e: e.matmul(pS[m][bf][:, 128 * r0:512], lhsT=KT[:, h, j * 128:(j + 1) * 128], rhs=Qp[s_][m][:, 128 * r0:512],
                                                          start=True, stop=(len(nears) == 0)), reads=[r_KT, r_Qp[s_][m]], writes=[r_pS[m][bf]])
                            for r in nears:
                                ty = Q0 + r - j
                                S.op('pe', lambda e: e.matmul(pS[m][bf][:, 128 * r:128 * r + 128], lhsT=identb[:], rhs=biasB[:, ty * 4 + h, :],
                                                              start=False, stop=(r == nears[-1])), reads=[r_bB] + RC, writes=[r_pS[m][bf]])

                    def emit_exp(j):
                        r0 = max(0, j - Q0)
                        bf = j % 2
                        bcol = (4 + h) if j < NT else h
                        for m in range(2):
                            S.op('act', lambda e: e.activation(out=Pt[m][bf][:, 128 * r0:512], in_=pS[m][bf][:, 128 * r0:512], func=AF.Exp,
                                                               bias=bfp[:, bcol:bcol + 1], scale=0.125),
                                 reads=[r_pS[m][bf], r_bfp], writes=[r_Pt[m][bf]])

                    def emit_AV(j):
                        r0 = max(0, j - Q0)
                        bf = j % 2
                        for r in range(r0, 4):
                            for m in range(2):
                                a = 2 * r + m
                                last = (j == Q0 + r) and a in (2, 5, 7)
                                S.op('pe', lambda e: e.matmul(acc(a), lhsT=Pt[m][bf][:, 128 * r:128 * r + 128], rhs=VV[:, j, h, 0:130],
                                                              start=(j == 0 and a % 3 == 0), stop=last, skip_group_check=True),
                                     reads=[r_Pt[m][bf], r_VV], writes=[r_pO[a // 3]])

                    NJ = Q0 + 4
                    emit_S(0)
                    for j in range(NJ):
                        if j + 1 < NJ:
                            emit_S(j + 1)
                        emit_exp(j)
                        emit_AV(j)
                    if stop_after == 1.7:
                        dbo = nc.dram_tensor("dbg_pO", [128, 3, 512], F32, kind="ExternalOutput").ap()
                        dbp = nc.dram_tensor("dbg_Pt", [128, 4, 512], BF16, kind="ExternalOutput").ap()
                        dsb = sbt(P2, "dsb", [128, 3, 512], F32); r_dsb = Reg()
                        for b_ in range(3):
                            S.op('act', lambda e: e.activation(out=dsb[:, b_, :], in_=pO[b_][:], func=AF.Copy), reads=[r_pO[b_]], pw=[r_dsb])
                        S.dma('sp', lambda e: e.dma_start(out=dbo, in_=dsb[:]), reads=[r_dsb], writes=[Reg()])
                        for m_ in range(2):
                            for b_ in range(2):
                                S.dma('sp', lambda e: e.dma_start(out=dbp[:, m_ * 2 + b_, :], in_=Pt[m_][b_][:]), reads=[r_Pt[m_][b_]], writes=[Reg()])
                        dbk = nc.dram_tensor("dbg_KT", [128, 4, 2 * SH], BF16, kind="ExternalOutput").ap()
                        dbq = nc.dram_tensor("dbg_Qp", [128, 2, 512], BF16, kind="ExternalOutput").ap()
                        S.dma('sp', lambda e: e.dma_start(out=dbk, in_=KT[:]), reads=[r_KT], writes=[Reg()])
                        for m_ in range(2):
                            S.dma('sp', lambda e: e.dma_start(out=dbq[:, m_, :], in_=Qp[s_][m_][:]), reads=[r_Qp[s_][m_]], writes=[Reg()])
                        S.barrier()
                        return nc, S
                    for r in range(4):
                        a0, a1 = 2 * r, 2 * r + 1
                        S.op('dve', lambda e: e.reciprocal(out=rz[:, 0:1], in_=acc(a0, 1, 128)), reads=[r_pO[a0 // 3]], writes=[r_rz])
                        S.op('dve', lambda e: e.reciprocal(out=rz[:, 1:2], in_=acc(a1, 1, 128)), reads=[r_pO[a1 // 3], r_rz], writes=[r_rz])
                        S.op('dve', lambda e: e.tensor_tensor(out=rz[:, 2:3], in0=rz[:, 1:2], in1=lamT[:, 5:6], op=ALU.mult), reads=[r_rz, r_lam], writes=[r_rz])
                        S.op('act', lambda e: e.activation(out=o1sb[:], in_=acc(a0, 128), func=AF.Copy, scale=rz[:, 0:1]), reads=[r_pO[a0 // 3], r_rz], writes=[r_o1])
                        S.op('dve', lambda e: e.scalar_tensor_tensor(out=odsb[:], in0=acc(a1, 128), scalar=rz[:, 2:3], in1=o1sb[:], op0=ALU.mult, op1=ALU.add),
                             reads=[r_pO[a1 // 3], r_rz, r_o1], writes=[r_od])
                        S.op('act', lambda e: e.activation(out=osq2[:], in_=odsb[:], func=AF.Square, accum_out=rz[:, 3:4]), reads=[r_od, r_rz], writes=[r_osq2, r_rz])
                        S.op('act', lambda e: e.activation(out=rz[:, 4:5], in_=rz[:, 3:4], func=AF.Sqrt, bias=epsT[:, 0:1], scale=1.0 / 128.0), reads=[r_rz] + RC, writes=[r_rz])
                        S.op('dve', lambda e: e.reciprocal(out=rz[:, 5:6], in_=rz[:, 4:5]), reads=[r_rz], writes=[r_rz])
                        S.op('dve', lambda e: e.scalar_tensor_tensor(out=hdt[:, r, :], in0=odsb[:], scalar=rz[:, 5:6], in1=dnwb[:], op0=ALU.mult, op1=ALU.mult),
                             reads=[r_od, r_rz, r_dnwb], pw=[r_hdt])
                    if stop_after == 1.8:
                        S.barrier()
                        return nc, S
                    for r in range(4):
                        S.dma('sp', lambda e: e.dma_start(out=HDs[(4 * g + r) * 128:(4 * g + r + 1) * 128, 512 + h * 128:512 + (h + 1) * 128], in_=hdt[:, r, :]),
                              reads=[r_hdt], pw=[r_hd[4 * g + r]])
                    if stop_after == 1.9:
                        S.barrier()
                        return nc, S
            S.barrier()
        if stop_after <= 2:
            return nc, S

        with ExitStack() as P3:
            wo = sbt(P3, "wo", [128, 8, D], BF16); r_wo = Reg()
            for dc in range(8):
                S.dma('pool', lambda e: e.dma_start(out=wo[:, dc, :], in_=w_out[dc * 128:(dc + 1) * 128, :]), pw=[r_wo])
            rw = sbt(P3, "rw", [128, 8, NEXP], F32); cload('sp', rw[:], router_w.rearrange("(c p) e -> p c e", p=128))
            rbc = sbt(P3, "rbc", [128, NEXP], F32); cload('sp', rbc[:], router_b.partition_broadcast(128))
            ecap = sbt(P3, "ecap", [128, NEXP], F32); cload('sp', ecap[:], c_ecap.partition_broadcast(128))
            sub = sbt(P3, "sub", [128, 128], BF16); cload('pool', sub[:], c_su)
            hb_2 = [sbt(P3, f"hb_{i_}", [128, D], BF16) for i_ in range(2)]; r_hb_2 = [Reg(), Reg()]
            hT_2 = [sbt(P3, f"hT_{i_}", [128, 8, 128], BF16) for i_ in range(2)]; r_hT_2 = [Reg(), Reg()]
            xf_2 = [sbt(P3, f"xf_{i_}", [128, D], F32) for i_ in range(2)]; r_xf_2 = [Reg(), Reg()]
            rr_2 = [sbt(P3, f"rr_{i_}", [128, D], F32) for i_ in range(2)]; r_rr_2 = [Reg(), Reg()]
            x1_2 = [sbt(P3, f"x1_{i_}", [128, D], F32) for i_ in range(2)]; r_x1t_2 = [Reg(), Reg()]
            x1b_2 = [sbt(P3, f"x1b_{i_}", [128, D], BF16) for i_ in range(2)]; r_x1b_2 = [Reg(), Reg()]
            x1T_2 = [sbt(P3, f"x1T_{i_}", [128, 8, 128], F32) for i_ in range(2)]; r_x1T_2 = [Reg(), Reg()]
            rt = sbt(P3, "rt", [128, 8, NEXP], F32); r_rt = Reg()
            t8 = sbt(P3, "t8", [128, 16], F32); r_t8 = Reg()
            mkb = sbt(P3, "mkb", [128, NEXP], BF16); r_mkb = Reg()
            cumb = sbt(P3, "cumb", [128, NEXP], BF16); r_cumb = Reg()
            pT3 = pst(P3, "pT3", [128, 1024], BF16); r_pT3 = Reg(True)
            pA = [pst(P3, f"pA{i}", [128, 512], F32) for i in range(2)]; r_pA = [Reg(True), Reg(True)]
            pB = [pst(P3, f"pB{i}", [128, 512], F32) for i in range(2)]; r_pB = [Reg(True), Reg(True)]
            pL = pst(P3, "pL", [128, 512], F32); r_pL = Reg(True)
            S.op('dve', lambda e: e.memset(rt[:, 6, :], 0.0), writes=[r_rt])
            S.op('dve', lambda e: e.memset(cumb[:], 0.0), writes=[r_cumb])

            def p3_load(i):
                S.dma('sp', lambda e: e.dma_start(out=hb_2[i % 2][:], in_=HDs[i * 128:(i + 1) * 128, :]), reads=[r_hd[i]], writes=[r_hb_2[i % 2]])
                S.dma('sp', lambda e: e.dma_start(out=xf_2[i % 2][:], in_=x_all[SH + i * 128:SH + (i + 1) * 128, :]), writes=[r_xf_2[i % 2]])

            def p3_tile(i):
                hb, r_hb = hb_2[i % 2], r_hb_2[i % 2]; hT, r_hT = hT_2[i % 2], r_hT_2[i % 2]; xf, r_xf = xf_2[i % 2], r_xf_2[i % 2]; rr, r_rr = rr_2[i % 2], r_rr_2[i % 2]; x1, r_x1t = x1_2[i % 2], r_x1t_2[i % 2]; x1b, r_x1b = x1b_2[i % 2], r_x1b_2[i % 2]; x1T, r_x1T = x1T_2[i % 2], r_x1T_2[i % 2];
                if i == 0:
                    p3_load(0)
                if i + 1 < NT:
                    p3_load(i + 1)
                for dc in range(8):
                    S.op('pe', lambda e: e.transpose(out=pT3[:, dc * 128:(dc + 1) * 128], in_=hb[:, dc * 128:(dc + 1) * 128], identity=identb[:]),
                         reads=[r_hb] + RC, writes=[r_pT3])
                S.op('act', lambda e: e.activation(out=hT[:], in_=R3(pT3[:]), func=AF.Copy), reads=[r_pT3], writes=[r_hT])
                for n in range(2):
                    for dc in range(8):
                        S.op('pe', lambda e: e.matmul(pA[n][:], lhsT=hT[:, dc, :], rhs=wo[:, dc, n * 512:(n + 1) * 512], start=(dc == 0), stop=(dc == 7)),
                             reads=[r_hT, r_wo], writes=[r_pA[n]])
                    S.op('dve', lambda e: e.scalar_tensor_tensor(out=rr[:, n * 512:(n + 1) * 512], in0=xf[:, n * 512:(n + 1) * 512], scalar=ALPHA,
                                                                 in1=pA[n][:], op0=ALU.mult, op1=ALU.add), reads=[r_xf, r_pA[n]], pw=[r_rr])
                yield
                layer_norm(rr, r_rr, 0, x1, r_x1t)
                S.dma('sp', lambda e: e.dma_start(out=X1s[i * 128:(i + 1) * 128, :], in_=x1[:]), reads=[r_x1t], writes=[r_x1[i]])
                S.op('act', lambda e: e.activation(out=x1b[:], in_=x1[:], func=AF.Copy), reads=[r_x1t], writes=[r_x1b])
                yield
                for dc in range(8):
                    S.op('pe', lambda e: e.transpose(out=pB[dc // 4][:, (dc % 4) * 128:(dc % 4 + 1) * 128], in_=x1[:, dc * 128:(dc + 1) * 128], identity=ident[:]),
                         reads=[r_x1t] + RC, writes=[r_pB[dc // 4]])
                S.op('act', lambda e: e.activation(out=x1T[:, 0:4, :], in_=R3(pB[0][:]), func=AF.Copy), reads=[r_pB[0]], pw=[r_x1T])
                S.op('dve', lambda e: e.tensor_copy(out=x1T[:, 4:8, :], in_=R3(pB[1][:])), reads=[r_pB[1]], pw=[r_x1T])
                yield
                for dc in range(8):
                    S.op('pe', lambda e: e.matmul(pL[:, 0:NEXP], lhsT=x1T[:, dc, :], rhs=rw[:, dc, :], start=(dc == 0), stop=(dc == 7)),
                         reads=[r_x1T] + RC, writes=[r_pL])
                S.op('dve', lambda e: e.tensor_tensor(out=rt[:, 0, :], in0=pL[:, 0:NEXP], in1=rbc[:], op=ALU.add), reads=[r_pL, r_rt] + RC, writes=[r_rt])
                S.op('dve', lambda e: e.max(out=t8[:, 0:8], in_=rt[:, 0, :]), reads=[r_rt], writes=[r_t8])
                S.op('dve', lambda e: e.tensor_scalar(out=rt[:, 1, :], in0=rt[:, 0, :], scalar1=t8[:, 3:4], scalar2=None, op0=ALU.is_ge), reads=[r_rt, r_t8], writes=[r_rt])
                S.op('dve', lambda e: e.tensor_scalar(out=t8[:, 8:9], in0=t8[:, 0:1], scalar1=-1.0, scalar2=None, op0=ALU.mult), reads=[r_t8], writes=[r_t8])
                S.op('act', lambda e: e.activation(out=rt[:, 2, :], in_=rt[:, 0, :], func=AF.Exp, bias=t8[:, 8:9]), reads=[r_rt, r_t8], writes=[r_rt])
                S.op('dve', lambda e: e.tensor_tensor(out=rt[:, 2, :], in0=rt[:, 2, :], in1=rt[:, 1, :], op=ALU.mult), reads=[r_rt], writes=[r_rt])
                S.op('act', lambda e: e.activation(out=rt[:, 5, :], in_=rt[:, 2, :], func=AF.Copy, accum_out=t8[:, 9:10]), reads=[r_rt, r_t8], writes=[r_rt, r_t8])
                S.op('dve', lambda e: e.reciprocal(out=t8[:, 10:11], in_=t8[:, 9:10]), reads=[r_t8], writes=[r_t8])
                S.op('dve', lambda e: e.tensor_scalar(out=rt[:, 3, :], in0=rt[:, 2, :], scalar1=t8[:, 10:11], scalar2=None, op0=ALU.mult), reads=[r_rt, r_t8], writes=[r_rt])
                S.op('dve', lambda e: e.tensor_copy(out=mkb[:], in_=rt[:, 1, :]), reads=[r_rt], writes=[r_mkb])
                S.op('pe', lambda e: e.matmul(pL[:, 64:64 + NEXP], lhsT=sub[:], rhs=mkb[:], start=True, stop=False), reads=[r_mkb] + RC, writes=[r_pL])
                S.op('pe', lambda e: e.matmul(pL[:, 64:64 + NEXP], lhsT=onesb[:], rhs=cumb[:], start=False, stop=True), reads=[r_cumb] + RC, writes=[r_pL])
                S.op('dve', lambda e: e.tensor_tensor(out=rt[:, 4, :], in0=pL[:, 64:64 + NEXP], in1=ecap[:], op=ALU.add), reads=[r_pL, r_rt] + RC, writes=[r_rt])
                S.op('dve', lambda e: e.tensor_tensor(out=rt[:, 4, :], in0=rt[:, 4, :], in1=rt[:, 1, :], op=ALU.mult), reads=[r_rt], writes=[r_rt])
                S.op('dve', lambda e: e.tensor_tensor(out=rt[:, 6, :], in0=rt[:, 6, :], in1=rt[:, 1, :], op=ALU.add), reads=[r_rt], writes=[r_rt])
                S.op('dve', lambda e: e.tensor_copy(out=cumb[:], in_=rt[:, 6, :]), reads=[r_rt], writes=[r_cumb])
                S.op('dve', lambda e: e.max(out=t8[:, 0:8], in_=rt[:, 4, :]), reads=[r_rt, r_t8], writes=[r_t8])
                S.op('dve', lambda e: e.tensor_scalar(out=posk[:, i, :], in0=t8[:, 0:4], scalar1=-1.0, scalar2=None, op0=ALU.add), reads=[r_t8], writes=[r_posk[i]])
                for k in range(4):
                    S.op('dve', lambda e: e.tensor_scalar(out=rt[:, 5, :], in0=rt[:, 4, :], scalar1=t8[:, k:k + 1], scalar2=None, op0=ALU.is_equal),
                         reads=[r_rt, r_t8], writes=[r_rt])
                    S.op('dve', lambda e: e.tensor_tensor(out=rt[:, 5, :], in0=rt[:, 5, :], in1=rt[:, 3, :], op=ALU.mult), reads=[r_rt], writes=[r_rt])
                    S.op('act', lambda e: e.activation(out=rt[:, 7, :], in_=rt[:, 5, :], func=AF.Copy, accum_out=gk[:, i, k:k + 1]), reads=[r_rt], writes=[r_rt], pw=[r_gk[i]])
                yield
                for k in range(4):
                    S.dma('pool', lambda e: e.indirect_dma_start(out=XSs, out_offset=bass.IndirectOffsetOnAxis(ap=posk[:, i, k:k + 1], axis=0),
                                                                in_=x1b[:], in_offset=None), reads=[r_x1b, r_posk[i]], pw=[r_xs])
            run_staggered(p3_tile, NT, 2)
            S.barrier()
        if stop_after <= 3:
            return nc, S

        NSG = -(-CAP // 512)
        SGW = CAP // NSG
        with ExitStack() as P4:
            wg = [sbt(P4, f"wg{i}", [128, 8, 2 * DEXP], BF16) for i in range(2)]; r_wg = [Reg(), Reg()]
            wd = [sbt(P4, f"wd{i}", [128, 8, D], BF16) for i in range(2)]; r_wd = [Reg(), Reg()]
            bdr = [sbt(P4, f"bdr{i}", [1, D], BF16) for i in range(2)]; r_bdr = [Reg(), Reg()]
            bgu = sbt(P4, "bgu", [128, NEXP * 16], F32); cload('sp', bgu[:], b_gu)
            xs = [sbt(P4, f"xs{i}", [128, D], BF16) for i in range(2)]; r_xsb = [Reg(), Reg()]
            xsT = sbt(P4, "xsT", [128, 8, CAP], BF16); r_xsT = Reg()
            actT = sbt(P4, "actT", [128, 8, CAP], BF16); r_actT = Reg()
            gsb_2 = [sbt(P4, f"gsb{i}", [128, SGW], F32) for i in range(2)]; r_gsb_2 = [Reg(), Reg()]
            ssb_2 = [sbt(P4, f"ssb{i}", [128, SGW], F32) for i in range(2)]; r_ssb_2 = [Reg(), Reg()]
            usb4_2 = [sbt(P4, f"usb4{i}", [128, SGW], F32) for i in range(2)]; r_usb4_2 = [Reg(), Reg()]
            ysb = [sbt(P4, f"ysb{i}", [128, D], BF16) for i in range(2)]; r_ysb = [Reg(), Reg()]
            pT4 = pst(P4, "pT4", [128, 1024], BF16); r_pT4 = Reg(True)
            pGt = [pst(P4, f"pGt{i}", [128, 512], F32) for i in range(2)]; r_pGt = [Reg(True), Reg(True)]
            pUp = [pst(P4, f"pUp{i}", [128, 512], F32) for i in range(2)]; r_pUp = [Reg(True), Reg(True)]
            pY = [pst(P4, f"pY{i}", [128, 512], F32) for i in range(2)]; r_pY = [Reg(True), Reg(True)]

            def load_w(e_):
                b_ = e_ % 2
                for dc in range(8):
                    S.dma('pool', lambda e: e.dma_start(out=wg[b_][:, dc, :], in_=w_gu[e_, dc * 128:(dc + 1) * 128, :]), pw=[r_wg[b_]])
                for dc in range(8):
                    S.dma('pool', lambda e: e.dma_start(out=wd[b_][:, dc, :], in_=w_dn[e_, dc * 128:(dc + 1) * 128, :]), pw=[r_wd[b_]])
                S.dma('pool', lambda e: e.dma_start(out=bdr[b_][:], in_=b_dn[e_:e_ + 1, :]), pw=[r_bdr[b_]])

            load_w(0)
            cnt4 = 0
            ycnt = 0
            for e_ in range(NEXP):
                b_ = e_ % 2
                if e_ + 1 < NEXP:
                    load_w(e_ + 1)
                for s_ in range(NSL):
                    xb_ = xs[s_ % 2]; rxb_ = r_xsb[s_ % 2]
                    S.dma('sp', lambda e: e.dma_start(out=xb_[:], in_=XSs[e_ * CAP + s_ * 128:e_ * CAP + (s_ + 1) * 128, :]), reads=[r_xs], writes=[rxb_])
                    for dc in range(8):
                        S.op('pe', lambda e: e.transpose(out=pT4[:, dc * 128:(dc + 1) * 128], in_=xb_[:, dc * 128:(dc + 1) * 128], identity=identb[:]),
                             reads=[rxb_] + RC, writes=[r_pT4])
                    S.op('act', lambda e: e.activation(out=xsT[:, :, s_ * 128:(s_ + 1) * 128], in_=R3(pT4[:]), func=AF.Copy), reads=[r_pT4], pw=[r_xsT])
                for c in range(8):
                    for sg in range(NSG):
                        sl = slice(sg * SGW, (sg + 1) * SGW)
                        pb_ = cnt4 % 2; cnt4 += 1
                        gsb, r_gsb = gsb_2[pb_], r_gsb_2[pb_]; ssb, r_ssb = ssb_2[pb_], r_ssb_2[pb_]; usb4, r_usb4 = usb4_2[pb_], r_usb4_2[pb_]
                        for dc in range(8):
                            S.op('pe', lambda e: e.matmul(pGt[pb_][:, 0:SGW], lhsT=wg[b_][:, dc, c * 128:(c + 1) * 128], rhs=xsT[:, dc, sl], start=(dc == 0), stop=(dc == 7)),
                                 reads=[r_wg[b_], r_xsT], writes=[r_pGt[pb_]])
                        for dc in range(8):
                            S.op('pe', lambda e: e.matmul(pUp[pb_][:, 0:SGW], lhsT=wg[b_][:, dc, DEXP + c * 128:DEXP + (c + 1) * 128], rhs=xsT[:, dc, sl], start=(dc == 0), stop=(dc == 7)),
                                 reads=[r_wg[b_], r_xsT], writes=[r_pUp[pb_]])
                        S.op('dve', lambda e: e.tensor_scalar(out=gsb[:], in0=pGt[pb_][:, 0:SGW], scalar1=bgu[:, e_ * 16 + c:e_ * 16 + c + 1], scalar2=7.0, op0=ALU.add, op1=ALU.min),
                             reads=[r_pGt[pb_]] + RC, writes=[r_gsb])
                        S.op('act', lambda e: e.activation(out=ssb[:], in_=gsb[:], func=AF.Silu, scale=1.702), reads=[r_gsb], writes=[r_ssb])
                        S.op('dve', lambda e: e.tensor_scalar(out=usb4[:], in0=pUp[pb_][:, 0:SGW], scalar1=bgu[:, e_ * 16 + 8 + c:e_ * 16 + 8 + c + 1], scalar2=7.0, op0=ALU.add, op1=ALU.min),
                             reads=[r_pUp[pb_]] + RC, writes=[r_usb4])
                        S.op('dve', lambda e: e.tensor_scalar(out=usb4[:], in0=usb4[:], scalar1=-7.0, scalar2=1.0, op0=ALU.max, op1=ALU.add), reads=[r_usb4], writes=[r_usb4])
                        S.op('dve', lambda e: e.scalar_tensor_tensor(out=actT[:, c, sl], in0=ssb[:], scalar=1.0 / 1.702, in1=usb4[:], op0=ALU.mult, op1=ALU.mult), reads=[r_ssb, r_usb4], pw=[r_actT])
                for s_ in range(NSL):
                    yb_ = ysb[ycnt % 2]; ryb_ = r_ysb[ycnt % 2]; ycnt += 1
                    for n in range(2):
                        for c in range(8):
                            S.op('pe', lambda e: e.matmul(pY[n][:], lhsT=actT[:, c, s_ * 128:(s_ + 1) * 128], rhs=wd[b_][:, c, n * 512:(n + 1) * 512], start=(c == 0), stop=False),
                                 reads=[r_actT, r_wd[b_]], writes=[r_pY[n]])
                        S.op('pe', lambda e: e.matmul(pY[n][:], lhsT=onesb[0:1, :], rhs=bdr[b_][0:1, n * 512:(n + 1) * 512], start=False, stop=True),
                             reads=[r_bdr[b_]] + RC, writes=[r_pY[n]])
                    S.op('act', lambda e: e.activation(out=yb_[:, 0:512], in_=pY[0][:], func=AF.Copy), reads=[r_pY[0]], pw=[ryb_])
                    S.op('dve', lambda e: e.tensor_copy(out=yb_[:, 512:1024], in_=pY[1][:]), reads=[r_pY[1]], pw=[ryb_])
                    S.dma('sp', lambda e: e.dma_start(out=YSs[e_ * CAP + s_ * 128:e_ * CAP + (s_ + 1) * 128, :], in_=yb_[:]), reads=[ryb_], pw=[r_ys])
            S.barrier()
        if stop_after <= 4:
            return nc, S

        with ExitStack() as P5:
            wgt = sbt(P5, "wgt", [128, 8, D], BF16); r_wgt = Reg()
            for dc in range(8):
                S.dma('pool', lambda e: e.dma_start(out=wgt[:, dc, :], in_=pg_w[dc * 128:(dc + 1) * 128, :]), pw=[r_wgt])
            wpp = sbt(P5, "wpp", [128, 2, D], BF16)
            for dc in range(2):
                S.dma('pool', lambda e: e.dma_start(out=wpp[:, dc, :], in_=pp_w[dc * 128:(dc + 1) * 128, :]), pw=[r_wgt])
            pgbr = sbt(P5, "pgbr", [1, D], BF16)
            S.dma('pool', lambda e: e.dma_start(out=pgbr[:], in_=pg_b), pw=[r_wgt])
            yk_2 = [[sbt(P5, f"yk{k}_{i_}", [128, D], BF16) for k in range(4)] for i_ in range(2)]; r_yk_2 = [[Reg() for _ in range(4)] for _ in range(2)]
            x1t_2 = [sbt(P5, f"x1t_{i_}", [128, D], F32) for i_ in range(2)]; r_x1r_2 = [Reg(), Reg()]
            rr5_2 = [sbt(P5, f"rr5_{i_}", [128, D], F32) for i_ in range(2)]; r_rr5_2 = [Reg(), Reg()]
            x2_2 = [sbt(P5, f"x2_{i_}", [128, D], F32) for i_ in range(2)]; r_x2_2 = [Reg(), Reg()]
            x2b_2 = [sbt(P5, f"x2b_{i_}", [128, D], BF16) for i_ in range(2)]; r_x2b_2 = [Reg(), Reg()]
            x2T_2 = [sbt(P5, f"x2T_{i_}", [128, 8, 128], BF16) for i_ in range(2)]; r_x2T_2 = [Reg(), Reg()]
            sgs_2 = [sbt(P5, f"sgs_{i_}", [128, D], F32) for i_ in range(2)]; r_sgs_2 = [Reg(), Reg()]
            pbt_2 = [sbt(P5, f"pbt_{i_}", [128, PLE], BF16) for i_ in range(2)]; r_pbt_2 = [Reg(), Reg()]
            pTt_2 = [sbt(P5, f"pTt_{i_}", [128, 2, 128], BF16) for i_ in range(2)]; r_pTt_2 = [Reg(), Reg()]
            r3_2 = [sbt(P5, f"r3_{i_}", [128, D], F32) for i_ in range(2)]; r_r3_2 = [Reg(), Reg()]
            ot_2 = [sbt(P5, f"ot_{i_}", [128, D], F32) for i_ in range(2)]; r_ot_2 = [Reg(), Reg()]
            pT5 = pst(P5, "pT5", [128, 1024], BF16); r_pT5 = Reg(True)
            pA5 = [pst(P5, f"pA5{i}", [128, 512], F32) for i in range(2)]; r_pA5 = [Reg(True), Reg(True)]
            pB5 = [pst(P5, f"pB5{i}", [128, 512], F32) for i in range(2)]; r_pB5 = [Reg(True), Reg(True)]
            def p5_load(i):
                for k in range(4):
                    S.dma('pool', lambda e: e.indirect_dma_start(out=yk_2[i % 2][k][:], out_offset=None, in_=YSs,
                                                                in_offset=bass.IndirectOffsetOnAxis(ap=posk[:, i, k:k + 1], axis=0)),
                          reads=[r_ys, r_posk[i]], writes=[r_yk_2[i % 2][k]])
                S.dma('sp', lambda e: e.dma_start(out=x1t_2[i % 2][:], in_=X1s[i * 128:(i + 1) * 128, :]), reads=[r_x1[i]], writes=[r_x1r_2[i % 2]])
                S.dma('pool', lambda e: e.dma_start(out=pbt_2[i % 2][:], in_=p_own[i * 128:(i + 1) * 128, :]), writes=[r_pbt_2[i % 2]])

            def p5_tile(i):
                yk, r_yk = yk_2[i % 2], r_yk_2[i % 2]
                x1t, r_x1r = x1t_2[i % 2], r_x1r_2[i % 2]; rr5, r_rr5 = rr5_2[i % 2], r_rr5_2[i % 2]; x2, r_x2 = x2_2[i % 2], r_x2_2[i % 2]; x2b, r_x2b = x2b_2[i % 2], r_x2b_2[i % 2]; x2T, r_x2T = x2T_2[i % 2], r_x2T_2[i % 2]; sgs, r_sgs = sgs_2[i % 2], r_sgs_2[i % 2]; pbt, r_pbt = pbt_2[i % 2], r_pbt_2[i % 2]; pTt, r_pTt = pTt_2[i % 2], r_pTt_2[i % 2]; r3, r_r3 = r3_2[i % 2], r_r3_2[i % 2]; ot, r_ot = ot_2[i % 2], r_ot_2[i % 2];
                if i == 0:
                    p5_load(0)
                if i + 1 < NT:
                    p5_load(i + 1)
                S.op('act', lambda e: e.activation(out=rr5[:], in_=x1t[:], func=AF.Copy, scale=ALPHA), reads=[r_x1r], writes=[r_rr5])
                for k in range(4):
                    S.op('dve', lambda e: e.scalar_tensor_tensor(out=rr5[:], in0=yk[k][:], scalar=gk[:, i, k:k + 1], in1=rr5[:], op0=ALU.mult, op1=ALU.add),
                         reads=[r_yk[k], r_gk[i], r_rr5], writes=[r_rr5])
                yield
                layer_norm(rr5, r_rr5, 1, x2, r_x2)
                S.op('act', lambda e: e.activation(out=x2b[:], in_=x2[:], func=AF.Copy), reads=[r_x2], writes=[r_x2b])
                yield
                for dc in range(8):
                    S.op('pe', lambda e: e.transpose(out=pT5[:, dc * 128:(dc + 1) * 128], in_=x2b[:, dc * 128:(dc + 1) * 128], identity=identb[:]),
                         reads=[r_x2b] + RC, writes=[r_pT5])
                S.op('act', lambda e: e.activation(out=x2T[:], in_=R3(pT5[:]), func=AF.Copy), reads=[r_pT5], writes=[r_x2T])
                for dc in range(2):
                    S.op('pe', lambda e: e.transpose(out=pT5[:, dc * 128:(dc + 1) * 128], in_=pbt[:, dc * 128:(dc + 1) * 128], identity=identb[:]),
                         reads=[r_pbt] + RC, writes=[r_pT5])
                S.op('act', lambda e: e.activation(out=pTt[:], in_=R3(pT5[:, 0:256]), func=AF.Copy), reads=[r_pT5], writes=[r_pTt])
                yield
                for n in range(2):
                    for dc in range(8):
                        S.op('pe', lambda e: e.matmul(pA5[n][:], lhsT=x2T[:, dc, :], rhs=wgt[:, dc, n * 512:(n + 1) * 512], start=(dc == 0), stop=False),
                             reads=[r_x2T, r_wgt], writes=[r_pA5[n]])
                    S.op('pe', lambda e: e.matmul(pA5[n][:], lhsT=onesb[0:1, :], rhs=pgbr[0:1, n * 512:(n + 1) * 512], start=False, stop=True),
                         reads=[r_wgt] + RC, writes=[r_pA5[n]])
                    S.op('act', lambda e: e.activation(out=sgs[:, n * 512:(n + 1) * 512], in_=pA5[n][:], func=AF.Sigmoid), reads=[r_pA5[n]], pw=[r_sgs])
                    for dc in range(2):
                        S.op('pe', lambda e: e.matmul(pB5[n][:], lhsT=pTt[:, dc, :], rhs=wpp[:, dc, n * 512:(n + 1) * 512], start=(dc == 0), stop=(dc == 1)),
                             reads=[r_pTt, r_wgt], writes=[r_pB5[n]])
                    S.op('dve', lambda e: e.tensor_tensor(out=r3[:, n * 512:(n + 1) * 512], in0=pB5[n][:], in1=sgs[:, n * 512:(n + 1) * 512], op=ALU.mult),
                         reads=[r_pB5[n], r_sgs], pw=[r_r3])
                yield
                S.op('dve', lambda e: e.scalar_tensor_tensor(out=r3[:], in0=x2[:], scalar=ALPHA, in1=r3[:], op0=ALU.mult, op1=ALU.add),
                     reads=[r_x2, r_r3], writes=[r_r3])
                layer_norm(r3, r_r3, 2, ot, r_ot)
                S.dma('sp', lambda e: e.dma_start(out=y_out[i * 128:(i + 1) * 128, :], in_=ot[:]), reads=[r_ot], pw=[r_yout])
            run_staggered(p5_tile, NT, 2)
            S.barrier()
    return nc, S


def make_inputs(inputs, SH, CAP):
    f = lambda a: np.ascontiguousarray(np.asarray(a, dtype=np.float32))
    x = f(inputs['x']); p = f(inputs['p'])
    B = x.shape[0]
    cst = host_consts()
    shared = {
        'w_in': f(inputs['w_in'][0]), 'conv_w': np.ascontiguousarray(f(inputs['conv_w'][0]).reshape(4, 12, 128).transpose(2, 1, 0)),
        'gdn_a_log': f(inputs['gdn_a_log']), 'gdn_dt_bias': f(inputs['gdn_dt_bias']),
        'gdn_norm_w': f(inputs['gdn_norm_w']), 'diff_norm_w': f(inputs['diff_norm_w']),
        'diff_lq1': f(inputs['diff_lq1']), 'diff_lk1': f(inputs['diff_lk1']),
        'diff_lq2': f(inputs['diff_lq2']), 'diff_lk2': f(inputs['diff_lk2']),
        'w_out': f(inputs['w_out'][0]), 'rel_bias': f(inputs['rel_bias']).reshape(1, 128),
        'ln1_g': f(inputs['ln1_g']), 'ln1_b': f(inputs['ln1_b']), 'ln2_g': f(inputs['ln2_g']), 'ln2_b': f(inputs['ln2_b']),
        'ln3_g': f(inputs['ln3_g']), 'ln3_b': f(inputs['ln3_b']),
        'router_w': f(inputs['router_w'][0]), 'router_b': f(inputs['router_b']),
        'w_gate_up': f(inputs['w_gate_up'][0]), 'b_gate_up': np.ascontiguousarray(f(inputs['b_gate_up'][0]).reshape(NEXP * 16, 128).T),
        'w_down': f(inputs['w_down'][0]), 'b_down': f(inputs['b_down'][0]),
        'ple_gate_w': f(inputs['ple_gate_w'][0]), 'ple_gate_b': f(inputs['ple_gate_b']), 'ple_proj_w': f(inputs['ple_proj_w'][0]),
        'c_ident': cst['ident'], 'c_trichi': cst['trichi'], 'c_blk': cst['blk'], 'c_negt': cst['negt'], 'c_smask': cst['smask'],
        'c_su': cst['su'], 'c_t5oh': cst['t5oh'], 'c_negd': cst['negd'],
        'c_ecap': (np.arange(NEXP, dtype=np.float32) * CAP + 1.0).reshape(1, NEXP),
    }
    maps = []
    for c in range(2 * B):
        b, hf = c // 2, c % 2
        if hf == 0:
            xa = np.concatenate([np.zeros((SH, D), np.float32), x[b, :SH]], axis=0)
            pm = np.full((128, 1), NEG, np.float32)
        else:
            xa = x[b]
            pm = np.zeros((128, 1), np.float32)
        m = dict(shared)
        m['x_all'] = np.ascontiguousarray(xa)
        m['p_own'] = np.ascontiguousarray(p[0, b, hf * SH:(hf + 1) * SH])
        m['pmask'] = pm
        maps.append(m)
    return maps


_NC_CACHE = {}


def kernel(**inputs):
    x = np.asarray(inputs['x'])
    B, SEQ, _ = x.shape
    SH = SEQ // 2
    CAP = 768 if SH >= 4096 else 256
    key = (SH, CAP)
    if key not in _NC_CACHE:
        _NC_CACHE[key] = build(SH, CAP)[0]
    nc = _NC_CACHE[key]
    maps = make_inputs(inputs, SH, CAP)
    res = run_bass_kernel_spmd(nc, maps, core_ids=list(range(2 * B)))
    out = np.zeros((B, SEQ, D), np.float32)
    for c in range(2 * B):
        out[c // 2, (c % 2) * SH:(c % 2 + 1) * SH] = res.results[c]['y_out']
    return out
```
